# Optimizing a Trainium2 kernel written in Bass

```python
import math
import jax, jax.numpy as jnp
from jax import lax
import numpy as np

D_MODEL = 1024
BATCH = 16
SEQ = 2048
DEPTH = 1

HEAD_DIM = 64
ATTN_CONFIGS = ((128, 1), (512, 4), (2048, 16))
HEADS_PER_GROUP = 4
N_ATTN_HEADS = HEADS_PER_GROUP * len(ATTN_CONFIGS)
ATTN_WIDTH = N_ATTN_HEADS * HEAD_DIM
ATTN_OUT_WIDTH = HEADS_PER_GROUP * HEAD_DIM
BLOCK = 128
N_REL_BUCKETS = 32
REL_MAX_DISTANCE = 2048
NEG_INF = -1e30

POOL_SIZES = (2, 4, 8, 16)
POOL_GROUP_DIM = 128
POOL_WIDTH = POOL_GROUP_DIM * len(POOL_SIZES)

Q_OFF = 0
K_OFF = Q_OFF + ATTN_WIDTH
V_OFF = K_OFF + ATTN_WIDTH
POOL_OFF = V_OFF + ATTN_WIDTH
GATE_A_OFF = POOL_OFF + POOL_WIDTH
GATE_B_OFF = GATE_A_OFF + D_MODEL
IN_WIDTH = GATE_B_OFF + D_MODEL

N_EXPERT_GROUPS = 4
EXPERTS_PER_GROUP = 8
N_EXPERTS = N_EXPERT_GROUPS * EXPERTS_PER_GROUP
EXPERT_TOP_K = 2
D_EXPERT = 256

LN_EPS = 1e-5
DEEPNORM_ALPHA = (2.0 * DEPTH) ** 0.25
DEEPNORM_BETA = (8.0 * DEPTH) ** -0.25

kernel_name = 'hybrid_dilated_pool_hiermoe_block'


def layer_norm(x, gamma, beta):
    xf = x.astype(jnp.float32)
    mu = jnp.mean(xf, axis=-1, keepdims=True)
    var = jnp.mean(jnp.square(xf - mu), axis=-1, keepdims=True)
    return ((xf - mu) * lax.rsqrt(var + LN_EPS) * gamma + beta).astype(x.dtype)


def t5_causal_bucket(dist):
    max_exact = N_REL_BUCKETS // 2
    is_small = dist < max_exact
    d = jnp.maximum(dist, 1).astype(jnp.float32)
    large = max_exact + (jnp.log(d / max_exact) / math.log(REL_MAX_DISTANCE / max_exact)
                         * (N_REL_BUCKETS - max_exact)).astype(jnp.int32)
    large = jnp.minimum(large, N_REL_BUCKETS - 1)
    return jnp.where(is_small, dist, large)


def dilated_window_attention(q, k, v, bias_table, dilation, span):
    B, S, H, Dh = q.shape
    Z = B * dilation
    L = S // dilation
    nb = -(-L // BLOCK)
    Lp = nb * BLOCK

    def to_blocks(t):
        t = t.reshape(B, L, dilation, H, Dh).transpose(0, 2, 1, 3, 4).reshape(Z, L, H, Dh)
        t = jnp.pad(t, ((0, 0), (0, Lp - L), (0, 0), (0, 0)))
        return t.reshape(Z, nb, BLOCK, H, Dh)

    def with_prev(t):
        prev = jnp.pad(t[:, :-1], ((0, 0), (1, 0), (0, 0), (0, 0), (0, 0)))
        return jnp.concatenate([prev, t], axis=2)

    qb = to_blocks(q)
    kc = with_prev(to_blocks(k))
    vc = with_prev(to_blocks(v))
    logits = jnp.einsum('znqhd,znkhd->znhqk', qb, kc).astype(jnp.float32) * (Dh ** -0.5)

    qi = jnp.arange(BLOCK)[:, None]
    ki = jnp.arange(2 * BLOCK)[None, :]
    step = qi + BLOCK - ki
    in_window = (step >= 0) & (step <= span)
    bucket = t5_causal_bucket(jnp.clip(step, 0, span) * dilation)
    bias = jnp.transpose(bias_table[bucket], (2, 0, 1)).astype(jnp.float32)
    after_start = (jnp.arange(nb)[:, None, None] > 0) | (ki >= BLOCK)[None]
    valid = in_window[None] & after_start
    logits = jnp.where(valid[None, :, None], logits + bias[None, None], NEG_INF)

    m = jnp.max(logits, axis=-1, keepdims=True)
    p = jnp.exp(logits - m)
    s = jnp.sum(p, axis=-1, keepdims=True)
    o = jnp.einsum('znhqk,znkhd->znqhd', p.astype(v.dtype), vc)
    o = o / jnp.transpose(s, (0, 1, 3, 2, 4))
    lse = jnp.transpose((m + jnp.log(s))[..., 0], (0, 1, 3, 2))

    def from_blocks(t):
        t = t.reshape((Z, Lp) + t.shape[3:])[:, :L]
        t = t.reshape((B, dilation, L) + t.shape[2:])
        t = jnp.moveaxis(t, 1, 2)
        return t.reshape((B, S) + t.shape[3:])

    return from_blocks(o).astype(q.dtype), from_blocks(lse)


def dilated_attention_mixer(q, k, v, rel_bias_table):
    B, S = q.shape[:2]
    outs, lses = [], []
    for g, (window, dilation) in enumerate(ATTN_CONFIGS):
        hs = slice(g * HEADS_PER_GROUP, (g + 1) * HEADS_PER_GROUP)
        o, lse = dilated_window_attention(q[:, :, hs], k[:, :, hs], v[:, :, hs],
                                          rel_bias_table[:, hs], dilation, window // dilation)
        outs.append(o)
        lses.append(lse)
    weights = jax.nn.softmax(jnp.stack(lses, axis=0), axis=0)
    o = jnp.sum(weights[..., None] * jnp.stack(outs, axis=0).astype(jnp.float32), axis=0)
    return o.reshape(B, S, ATTN_OUT_WIDTH).astype(q.dtype)


def multiscale_pool_mixer(u, w_pool, pool_scale):
    B, S, _ = u.shape
    ug = u.reshape(B, S, len(POOL_SIZES), POOL_GROUP_DIM)
    csum = jnp.pad(jnp.cumsum(ug.astype(jnp.float32), axis=1), ((0, 0), (1, 0), (0, 0), (0, 0)))
    t = jnp.arange(S)
    pooled = []
    for gi, w in enumerate(POOL_SIZES):
        start = jnp.maximum(t + 1 - w, 0)
        count = jnp.minimum(t + 1, w).astype(jnp.float32)
        pooled.append((csum[:, t + 1, gi] - csum[:, start, gi]) / count[None, :, None])
    diff = (jnp.stack(pooled, axis=2) - ug.astype(jnp.float32)).astype(u.dtype)
    y = jnp.einsum('bsgc,gcd->bsgd', diff, w_pool).reshape(B, S, POOL_WIDTH)
    return y * pool_scale


def hierarchical_moe(h, w_router_group, b_router_group, w_router_expert, b_router_expert,
                     w_expert_gate, w_expert_up, w_expert_down):
    B, S, D = h.shape
    hf = h.reshape(B * S, D)
    group_probs = jax.nn.softmax((hf @ w_router_group + b_router_group).astype(jnp.float32), axis=-1)
    g_prob, g_idx = lax.top_k(group_probs, 1)
    expert_logits = (hf @ w_router_expert + b_router_expert).astype(jnp.float32)
    expert_logits = expert_logits.reshape(-1, N_EXPERT_GROUPS, EXPERTS_PER_GROUP)
    in_group = jnp.take_along_axis(expert_logits, g_idx[:, :, None], axis=1)[:, 0]
    top_logits, top_idx = lax.top_k(in_group, EXPERT_TOP_K)
    top_w = jax.nn.softmax(top_logits, axis=-1) * g_prob
    expert_id = g_idx * EXPERTS_PER_GROUP + top_idx
    combine = jnp.sum(jax.nn.one_hot(expert_id, N_EXPERTS, dtype=jnp.float32) * top_w[..., None],
                      axis=1).astype(h.dtype)
    y = jnp.zeros_like(hf)
    for e in range(N_EXPERTS):
        hidden = jax.nn.silu(hf @ w_expert_gate[e]) * (hf @ w_expert_up[e])
        y = y + combine[:, e:e + 1] * (hidden @ w_expert_down[e])
    return y.reshape(B, S, D)


def setup_inputs(seed: int = 0) -> dict:
    key = jax.random.key(seed)
    ks = jax.random.split(key, 22)
    f32 = jnp.float32
    nrm = lambda k, shape, scale: jax.random.normal(k, shape, f32) * scale
    L = DEPTH
    col_scale = jnp.ones((IN_WIDTH,), f32).at[V_OFF:V_OFF + ATTN_WIDTH].set(DEEPNORM_BETA)
    return {
        'x': jax.random.normal(ks[0], (BATCH, SEQ, D_MODEL), f32),
        'w_in': nrm(ks[1], (L, D_MODEL, IN_WIDTH), D_MODEL ** -0.5) * col_scale,
        'b_in': nrm(ks[2], (L, IN_WIDTH), 0.02),
        'rel_bias_table': nrm(ks[3], (N_REL_BUCKETS, N_ATTN_HEADS), 0.5),
        'w_pool': nrm(ks[4], (L, len(POOL_SIZES), POOL_GROUP_DIM, POOL_GROUP_DIM), POOL_GROUP_DIM ** -0.5),
        'pool_scale': 1.0 + nrm(ks[5], (L, POOL_WIDTH), 0.1),
        'w_proj_attn': nrm(ks[6], (L, ATTN_OUT_WIDTH, D_MODEL), ATTN_OUT_WIDTH ** -0.5),
        'w_proj_pool': nrm(ks[7], (L, POOL_WIDTH, D_MODEL), POOL_WIDTH ** -0.5),
        'w_out': nrm(ks[8], (L, D_MODEL, D_MODEL), D_MODEL ** -0.5 * DEEPNORM_BETA),
        'ln1_gamma': 1.0 + nrm(ks[9], (L, D_MODEL), 0.05),
        'ln1_beta': nrm(ks[10], (L, D_MODEL), 0.02),
        'w_router_group': nrm(ks[11], (L, D_MODEL, N_EXPERT_GROUPS), D_MODEL ** -0.5),
        'b_router_group': nrm(ks[12], (L, N_EXPERT_GROUPS), 0.01),
        'w_router_expert': nrm(ks[13], (L, D_MODEL, N_EXPERTS), D_MODEL ** -0.5),
        'b_router_expert': nrm(ks[14], (L, N_EXPERTS), 0.01),
        'w_expert_gate': nrm(ks[15], (L, N_EXPERTS, D_MODEL, D_EXPERT), D_MODEL ** -0.5 * DEEPNORM_BETA),
        'w_expert_up': nrm(ks[16], (L, N_EXPERTS, D_MODEL, D_EXPERT), D_MODEL ** -0.5 * DEEPNORM_BETA),
        'w_expert_down': nrm(ks[17], (L, N_EXPERTS, D_EXPERT, D_MODEL), D_EXPERT ** -0.5 * DEEPNORM_BETA),
        'ln2_gamma': 1.0 + nrm(ks[18], (L, D_MODEL), 0.05),
        'ln2_beta': nrm(ks[19], (L, D_MODEL), 0.02),
    }


def reference(x, w_in, b_in, rel_bias_table, w_pool, pool_scale, w_proj_attn, w_proj_pool, w_out,
              ln1_gamma, ln1_beta, w_router_group, b_router_group, w_router_expert, b_router_expert,
              w_expert_gate, w_expert_up, w_expert_down, ln2_gamma, ln2_beta):
    B, S, D = x.shape
    for layer in range(DEPTH):
        proj = x @ w_in[layer] + b_in[layer]
        q = proj[..., Q_OFF:K_OFF].reshape(B, S, N_ATTN_HEADS, HEAD_DIM)
        k = proj[..., K_OFF:V_OFF].reshape(B, S, N_ATTN_HEADS, HEAD_DIM)
        v = proj[..., V_OFF:POOL_OFF].reshape(B, S, N_ATTN_HEADS, HEAD_DIM)
        u = proj[..., POOL_OFF:GATE_A_OFF]
        gate_a = jax.nn.sigmoid(proj[..., GATE_A_OFF:GATE_B_OFF])
        gate_b = jax.nn.sigmoid(proj[..., GATE_B_OFF:IN_WIDTH])

        y_attn = dilated_attention_mixer(q, k, v, rel_bias_table) @ w_proj_attn[layer]
        y_pool = multiscale_pool_mixer(u, w_pool[layer], pool_scale[layer]) @ w_proj_pool[layer]
        mixed = gate_a * y_attn + gate_b * y_pool
        x = layer_norm(DEEPNORM_ALPHA * x + mixed @ w_out[layer], ln1_gamma[layer], ln1_beta[layer])

        moe = hierarchical_moe(x, w_router_group[layer], b_router_group[layer],
                               w_router_expert[layer], b_router_expert[layer],
                               w_expert_gate[layer], w_expert_up[layer], w_expert_down[layer])
        x = layer_norm(DEEPNORM_ALPHA * x + moe, ln2_gamma[layer], ln2_beta[layer])
    return x
```

```python
import math
import numpy as np
import ml_dtypes
import concourse.bass as bass
import concourse.mybir as mybir
from concourse.bass_utils import run_bass_kernel_spmd

F32 = mybir.dt.float32
BF16 = mybir.dt.bfloat16
AF = mybir.ActivationFunctionType
ALU = mybir.AluOpType
AX = mybir.AxisListType

D = 1024
T = 2048
NT = 16
Q_OFF, K_OFF, V_OFF, POOL_OFF, GA_OFF, GB_OFF, IN_W = 0, 768, 1536, 2304, 2816, 3840, 4864
NEXP = 32
DEXP = 256
ALPHA = (2.0 * 1) ** 0.25
LN_EPS = 1e-5
DIL = (1, 4, 16)
N_CORES = 8
CUT = 99.0


class Res:
    __slots__ = ("name", "w", "rs")

    def __init__(self, name):
        self.name = name
        self.w = None
        self.rs = []


class Sched:
    ENG = ("pe", "act", "dve", "pool", "sp")

    def __init__(self):
        self.ops = {e: [] for e in self.ENG}
        self.seen = {e: {} for e in self.ENG}
        self.clock = {}
        self.chan = {}
        self.pending = {e: [] for e in self.ENG}

    def _need(self, eng, tok, waits):
        key, val = tok
        if key == eng and eng == "pe":
            return
        if self.seen[eng].get(key, -1) >= val:
            return
        waits.append(tok)
        if key in self.ENG:
            self.ops[key][val]["sig"] = True
        sn = self.seen[eng]
        for k, v in self.clock[tok].items():
            if sn.get(k, -1) < v:
                sn[k] = v

    def op(self, eng, fn, reads=(), writes=(), chan=None):
        waits = []
        toks = self.pending[eng]
        self.pending[eng] = []
        for r in reads:
            if r.w is not None:
                toks.append(r.w)
        for w in writes:
            if w.w is not None:
                toks.append(w.w)
            toks.extend(w.rs)
        for t in toks:
            self._need(eng, t, waits)
        idx = len(self.ops[eng])
        self.ops[eng].append(dict(fn=fn, waits=waits, sig=False, chan=chan))
        if chan is None:
            tok = (eng, idx)
        else:
            self.chan[chan] = self.chan.get(chan, 0) + 1
            tok = (chan, self.chan[chan])
        snap = dict(self.seen[eng])
        if snap.get(tok[0], -1) < tok[1]:
            snap[tok[0]] = tok[1]
        self.clock[tok] = snap
        for r in reads:
            r.rs.append(tok)
        for w in writes:
            w.w = tok
            w.rs = []
        return tok

    def barrier(self):
        toks = []
        for e in self.ENG:
            for i in range(len(self.ops[e]) - 1, -1, -1):
                if self.ops[e][i]["chan"] is None:
                    toks.append((e, i))
                    break
        for c, v in self.chan.items():
            toks.append((c, v))
        for e in self.ENG:
            self.pending[e].extend(toks)

    def emit(self, nc):
        trailing = {}
        for e in self.ENG:
            w = []
            for t in self.pending[e]:
                if t[0] == e:
                    continue
                self._need(e, t, w)
            self.pending[e] = []
            trailing[e] = w
        sems = {}
        for e in self.ENG:
            if e != "sp":
                sems[e] = nc.alloc_semaphore("sem_" + e)
        for c in self.chan:
            sems[c] = nc.alloc_semaphore("dma_" + c)
        sigcount = {}
        for e in self.ENG:
            cnt = 0
            m = []
            for rec in self.ops[e]:
                if rec["sig"]:
                    cnt += 1
                m.append(cnt)
            sigcount[e] = m
        self.sig_totals = {e: (sigcount[e][-1] if sigcount[e] else 0) for e in self.ENG}

        def run(name, eng):
            def wait(tok):
                key, val = tok
                if key in self.ENG:
                    eng.wait_ge(sems[key], sigcount[key][val])
                else:
                    eng.wait_ge(sems[key], 16 * val)

            for rec in self.ops[name]:
                for tok in rec["waits"]:
                    wait(tok)
                ins = rec["fn"](eng)
                if rec["chan"] is not None:
                    ins.then_inc(sems[rec["chan"]], 16)
                elif rec["sig"]:
                    ins.then_inc(sems[name], 1)
            for tok in trailing[name]:
                wait(tok)

        with nc.Block() as block:
            @block.sync
            def _(e):
                run("sp", e)

            @block.tensor
            def _(e):
                run("pe", e)

            @block.scalar
            def _(e):
                run("act", e)

            @block.vector
            def _(e):
                run("dve", e)

            @block.gpsimd
            def _(e):
                run("pool", e)


def tokset(g, j):
    if g == 0:
        return slice(128 * j, 128 * j + 128)
    if g == 1:
        r, n = j // 4, j % 4
        return slice(512 * n + r, 512 * (n + 1), 4)
    return slice(j, T, 16)


def prev_tile(g, j):
    if g == 0:
        return j - 1 if j > 0 else None
    if g == 1:
        return j - 1 if (j % 4) > 0 else None
    return None


def build_program(nb=2, stage=99, dbg=False):
    nc = bass.Bass("TRN2", target_bir_lowering=False)
    S = Sched()

    def din(name, shape, dt=F32):
        return nc.dram_tensor(name, list(shape), dt, kind="ExternalInput")

    x = din("x", [2, T, D])
    w_in = din("w_in", [D, IN_W])
    bcol_d = din("bcol", [128, 38])
    bvrow_d = din("bvrow", [1, 768])
    biasg_d = din("biasg", [128, 3 * 2 * 4 * 128])
    maskg_d = din("maskg", [128, 3 * 2 * 4 * 128])
    ident_d = din("ident", [128, 128])
    invc_d = din("invc", [128, 16])
    wpool_d = din("w_pool", [4, 128, 128])
    pscale_d = din("pscale", [128, 4])
    pa_d = din("w_proj_attn", [256, D])
    pb_d = din("w_proj_pool", [512, D])
    wout_d = din("w_out", [D, D])
    g1_d, b1_d = din("ln1_gamma", [D]), din("ln1_beta", [D])
    g2_d, b2_d = din("ln2_gamma", [D]), din("ln2_beta", [D])
    wr_d = din("w_router", [D, 36])
    brrow_d = din("brrow", [1, 36])
    nexp_decl = NEXP if stage >= 4 else 1
    weg_d = din("w_expert_gate", [nexp_decl, D, DEXP])
    weu_d = din("w_expert_up", [nexp_decl, D, DEXP])
    wed_d = din("w_expert_down", [nexp_decl, DEXP, D])
    out = nc.dram_tensor("out", [2, T, D], F32, kind="ExternalOutput")
    x1s = nc.dram_tensor("x1s", [T, D], F32, kind="ExternalOutput" if dbg else "Internal")
    dbg_o = {}
    if dbg:
        dbg_o["AT"] = nc.dram_tensor("dbg_AT", [128, 2 * T], BF16, kind="ExternalOutput")
        dbg_o["BT"] = nc.dram_tensor("dbg_BT", [128, 4 * T], BF16, kind="ExternalOutput")
        dbg_o["call"] = nc.dram_tensor("dbg_call", [128, NT * 32], F32, kind="ExternalOutput")
        dbg_o["moe"] = nc.dram_tensor("dbg_moe", [NT, 128, D], F32, kind="ExternalOutput")

    w_in_v = w_in.ap().rearrange("(c p) n -> p c n", p=128)

    def sb(name, shape, dt):
        return nc.alloc_sbuf_tensor("sb_" + name, list(shape), dt)

    ident = sb("ident", [128, 128], BF16)
    ones = sb("ones", [128, 128], BF16)
    bcol = sb("bcol", [128, 38], F32)
    bq8 = sb("bq8", [128, 6], F32)
    bvrow = sb("bvrow", [1, 768], BF16)
    pscale = sb("pscale", [128, 4], F32)
    invc = sb("invc", [128, 16], F32)
    gam1, bet1 = sb("gam1", [128, D], F32), sb("bet1", [128, D], F32)
    gam2, bet2 = sb("gam2", [128, D], F32), sb("bet2", [128, D], F32)
    Wout = sb("Wout", [128, 8, D], BF16)
    Pa = sb("Pa", [128, 2, D], BF16)
    Pb = sb("Pb", [128, 4, D], BF16)
    wpool = sb("wpool", [128, 4, 128], BF16)
    Wr = sb("Wr", [128, 8, 36], BF16)
    brrow = sb("brrow", [1, 36], BF16)
    stats = sb("stats", [128, 4, 2, 6], F32)
    mv = sb("mv", [128, 4, 2], F32)
    rstd = sb("rstd", [128, 4, 2], F32)
    tmp16 = sb("tmp16", [128, 16], F32)
    epsc = sb("epsc", [128, 1], F32)

    ARENA_KB = 156
    arena = nc.alloc_sbuf_tensor("arena", [128, ARENA_KB * 256], F32)

    def carve(off_kb, shape, dt):
        nbytes = int(np.prod(shape[1:])) * (4 if dt == F32 else 2)
        w0 = int(off_kb * 256)
        assert abs(w0 - off_kb * 256) < 1e-9 and nbytes % 4 == 0
        assert w0 * 4 + nbytes <= ARENA_KB * 1024, (off_kb, shape)
        ap = arena[:, w0:w0 + nbytes // 4]
        if dt != F32:
            ap = ap.bitcast(dt)
        if len(shape) == 3:
            ap = ap.rearrange("p (a b) -> p a b", a=shape[1])
        elif len(shape) == 4:
            ap = ap.rearrange("p (a b c) -> p a b c", a=shape[1], b=shape[2])
        elif len(shape) == 5:
            ap = ap.rearrange("p (a b c d) -> p a b c d", a=shape[1], b=shape[2], c=shape[3])
        return ap

    xT = carve(0, [128, 8, T], BF16)
    AT = carve(32, [128, 2, T], BF16)
    BT = carve(40, [128, 4, T], BF16)
    Emk = carve(40, [128, 3, 2, 512], F32)
    PT = [carve(52 + 2 * i, [128, 2, 512], BF16) for i in range(2)] + [carve(36, [128, 2, 512], BF16)]
    wq = [carve(56 + 12 * i, [128, 8, 768], BF16) for i in range(2)]
    wu = [carve(56 + 8 * i, [128, 8, 512], BF16) for i in range(2)]
    wg = [carve(56 + 8 * i, [128, 8, 2, 256], BF16) for i in range(2)]
    qk = [[carve(80 + 8 * j + 4 * p, [128, T], BF16) for p in range(2)] for j in range(2)]
    Vt = carve(96, [128, NT, 256], BF16)
    scr = [carve(104 + 4 * i, [128, 2, 512], F32) for i in range(2)] + [carve(32, [128, 2, 512], F32)]
    UTST = carve(112, [128, 4, T], F32)
    xbf = [carve(144 + 2 * i, [128, D], BF16) for i in range(4)]
    xst = [carve(128 + 4 * i, [128, D], F32) for i in range(4)]
    ubuf = [carve(72 + 8 * i, [128, T], F32) for i in range(2)]
    sbuf2 = [carve(88 + 8 * i, [128, T], F32) for i in range(2)]
    diffT = [carve(104 + 4 * i, [128, T], BF16) for i in range(2)]
    x1T = carve(72, [128, 8, T], BF16)
    G = [carve(104 + 4 * i, [128, 2, 512], F32) for i in range(2)]
    mixedT = carve(112, [128, 8, 512], BF16)
    t12 = [carve(120 + 4 * i, [128, 2, 512], F32) for i in range(2)]
    xtok = [carve(128 + 4 * i, [128, D], F32) for i in range(4)]
    x1bf = [carve(144 + 2 * i, [128, D], BF16) for i in range(2)] + [carve(153, [128, D], BF16)]
    logits = carve(148, [128, NT, 36], F32)
    rbig = carve(104, [128, 3, NT, 32], F32)
    rsm = carve(110, [128, 12, NT], F32)
    call = carve(150.5, [128, NT, 32], F32)
    acc = carve(0, [128, NT, D], F32)
    xtok2 = [carve(64 + 4 * i, [128, D], F32) for i in range(2)]
    wexp_g = [carve(104 + 12 * i, [128, 8, 256], BF16) for i in range(2)]
    wexp_u = [carve(108 + 12 * i, [128, 8, 256], BF16) for i in range(2)]
    wexp_d = [carve(112 + 12 * i, [128, 2, D], BF16) for i in range(2)]
    hT = [carve(128 + 2 * i, [128, 2, 512], BF16) for i in range(2)]
    sg = [carve(132 + 4 * i, [128, 2, 512], F32) for i in range(2)]

    banks = [nc.alloc_psum_tensor("bank%d" % i, [128, 512], F32) for i in range(8)]
    Rbank = [Res("bank%d" % i) for i in range(8)]

    def bank_bf(i):
        return banks[i][:, :].bitcast(BF16).rearrange("p (a b) -> p a b", a=8)

    def dma(eng, out_ap, in_ap, chan, reads=(), writes=()):
        return S.op(eng, lambda e: e.dma_start(out=out_ap, in_=in_ap), reads=reads, writes=writes, chan=chan + "_" + eng)

    dma("sp", bcol[:, :], bcol_d.ap(), "const")
    dma("sp", pscale[:, :], pscale_d.ap(), "const")
    dma("sp", invc[:, :], invc_d.ap(), "const")
    for t, d_ in ((gam1, g1_d), (bet1, b1_d), (gam2, g2_d), (bet2, b2_d)):
        dma("sp", t[:, :], d_.ap().partition_broadcast(128), "const")
    dma("pool", ident[:, :], ident_d.ap(), "const")
    dma("pool", bvrow[:, :], bvrow_d.ap(), "const")
    dma("pool", brrow[:, :], brrow_d.ap(), "const")
    dma("pool", Wout[:, :, :], wout_d.ap().rearrange("(c p) n -> p c n", p=128), "const")
    dma("pool", Pa[:, :, :], pa_d.ap().rearrange("(c p) n -> p c n", p=128), "const")
    dma("pool", Pb[:, :, :], pb_d.ap().rearrange("(c p) n -> p c n", p=128), "const")
    dma("pool", wpool[:, :, :], wpool_d.ap().rearrange("g c d -> c g d"), "const")
    dma("pool", Wr[:, :, :], wr_d.ap().rearrange("(c p) n -> p c n", p=128), "const")
    S.op("dve", lambda e: e.memset(ones[:, :], 1.0))
    S.op("dve", lambda e: e.memset(epsc[:, :], LN_EPS))
    S.barrier()
    S.op("dve", lambda e: e.tensor_scalar(bq8[:, :], bcol[:, 0:6], 0.125, None, ALU.mult))
    S.barrier()

    wslot = [0]
    Rw = [Res("w0"), Res("w1")]
    ipb = [0]

    def next_ipbank():
        ipb[0] ^= 1
        return 1 + ipb[0]

    for b in range(nb):
        if CUT <= 0.1:
            break
        R_xbf = [Res("xbf%d" % i) for i in range(4)]
        R_xT = [Res("xT%d" % i) for i in range(NT)]
        R_E = Res("E")
        R_scr = [Res("scr0"), Res("scr1")]
        R_scrh = [[Res("scrh%d%d" % (i, h)) for h in range(2)] for i in range(3)]
        R_PT = [[Res("PT%d%d" % (i, h)) for h in range(2)] for i in range(3)]
        R_UG = [[Res("UG%d_%d" % (g_, i)) for i in range(NT)] for g_ in range(3)]
        R_qk = [[Res("qk%d%d" % (j, p)) for p in range(2)] for j in range(2)]
        R_V = [Res("V%d" % i) for i in range(NT)]
        R_UTST = Res("UTST")
        R_AT = Res("AT")
        tpv = bank_bf(0)
        R_xst = [Res("xst%d" % i) for i in range(4)]
        for i in range(NT):
            sl = i % 4
            dma("sp", xst[sl][:, :], x[b, i * 128:(i + 1) * 128, :], "xst%d" % sl, writes=[R_xst[sl]])
            S.op("dve", lambda e, sl=sl: e.tensor_copy(xbf[sl][:, :], xst[sl][:, :]), reads=[R_xst[sl]], writes=[R_xbf[sl]])
            for c in range(8):
                S.op("pe", lambda e, c=c, sl=sl: e.transpose(tpv[:, c, :], xbf[sl][:, c * 128:(c + 1) * 128], ident[:, :]),
                     reads=[R_xbf[sl]], writes=[Rbank[0]])
            S.op("act", lambda e, i=i: e.copy(out=xT[:, :, i * 128:(i + 1) * 128], in_=tpv),
                 reads=[Rbank[0]], writes=[R_xT[i]])

        if CUT <= 0.2:
            break
        for g in range(3):
            for kb in range(2):
                o = (g * 2 + kb) * 512
                sc = scr[kb][:, 0, :]
                dma("sp", sc, biasg_d[:, o:o + 512], "escr%d" % kb, writes=[R_scrh[kb][0]])
                dma("sp", Emk[:, g, kb, :], maskg_d[:, o:o + 512], "emask", writes=[R_E])
                S.op("act", lambda e, sc=sc: e.activation(out=sc, in_=sc, func=AF.Exp),
                     reads=[R_scrh[kb][0]], writes=[R_scrh[kb][0]])
                S.op("dve", lambda e, sc=sc, g=g, kb=kb: e.tensor_tensor(Emk[:, g, kb, :], Emk[:, g, kb, :], sc, ALU.mult),
                     reads=[R_scrh[kb][0], R_E], writes=[R_E])

        att_ctr = [0]
        wslot[0] = 0
        if CUT <= 0.3:
            break
        for g in range(3 if CUT > 0.9 else 1):
            sl = wslot[0] % 2
            wslot[0] += 1
            for j, off in enumerate((Q_OFF, K_OFF, V_OFF)):
                dma("pool", wq[sl][:, :, j * 256:(j + 1) * 256],
                    w_in_v[:, :, off + g * 256: off + (g + 1) * 256], "w%d" % sl, writes=[Rw[sl]])
            for j in range(2):
                for p in range(2):
                    for tc in range(4):
                        bk = next_ipbank()
                        for c in range(8):
                            S.op("pe", lambda e, bk=bk, c=c, j=j, p=p, tc=tc, sl=sl: e.matmul(
                                banks[bk][:, :], wq[sl][:, c, j * 256 + p * 128: j * 256 + (p + 1) * 128],
                                xT[:, c, tc * 512:(tc + 1) * 512], start=(c == 0), stop=(c == 7)),
                                reads=[Rw[sl]] + R_xT[4 * tc:4 * tc + 4], writes=[Rbank[bk]])
                        col = (Q_OFF if j == 0 else K_OFF) // 128 + g * 2 + p
                        if j == 0:
                            S.op("act", lambda e, bk=bk, p=p, tc=tc, col=col: e.activation(
                                out=qk[0][p][:, tc * 512:(tc + 1) * 512], in_=banks[bk][:, :], func=AF.Identity,
                                bias=bq8[:, col:col + 1], scale=0.125), reads=[Rbank[bk]], writes=[R_qk[0][p]])
                        else:
                            S.op("act", lambda e, bk=bk, p=p, tc=tc, col=col: e.activation(
                                out=qk[1][p][:, tc * 512:(tc + 1) * 512], in_=banks[bk][:, :], func=AF.Identity,
                                bias=bcol[:, col:col + 1], scale=1.0), reads=[Rbank[bk]], writes=[R_qk[1][p]])
            for jt in range(NT):
                bk = next_ipbank()
                ts = tokset(g, jt)
                for c in range(8):
                    S.op("pe", lambda e, bk=bk, c=c, ts=ts, sl=sl: e.matmul(
                        banks[bk][:, 0:256], xT[:, c, ts], wq[sl][:, c, 512:768], start=(c == 0), stop=False),
                        reads=[Rw[sl]] + R_xT, writes=[Rbank[bk]])
                S.op("pe", lambda e, bk=bk, g=g: e.matmul(
                    banks[bk][:, 0:256], ones[0:1, :], bvrow[0:1, g * 256:(g + 1) * 256], start=False, stop=True),
                    writes=[Rbank[bk]])
                S.op("dve", lambda e, bk=bk, jt=jt: e.tensor_copy(Vt[:, jt, :], banks[bk][:, 0:256]),
                     reads=[Rbank[bk]], writes=[R_V[jt]])

            if g == 2:
                dma("pool", wu[0][:, :, :], w_in_v[:, :, POOL_OFF:POOL_OFF + 512], "w0", writes=[Rw[0]])
            if CUT <= 0.5:
                break

            def att_front(jt, g=g):
                ab = att_ctr[0] % 3
                sbk = (3, 4) if att_ctr[0] % 2 == 0 else (5, 6)
                ubk = (7, 0, 1)[ab]
                att_ctr[0] += 1
                qs = tokset(g, jt)
                kbs = []
                pj = prev_tile(g, jt)
                if pj is not None:
                    kbs.append((0, pj))
                kbs.append((1, jt))
                c0 = 0 if len(kbs) == 2 else 256
                for kb, jk in kbs:
                    ks = tokset(g, jk)
                    for p in range(2):
                        for h in range(2):
                            cb = (kb * 2 + p) * 128
                            S.op("pe", lambda e, cb=cb, ks=ks, qs=qs, p=p, h=h, sbk=sbk: e.matmul(
                                banks[sbk[h]][:, cb:cb + 128], qk[1][p][64 * h:64 * h + 64, ks],
                                qk[0][p][64 * h:64 * h + 64, qs], start=True, stop=True),
                                reads=[R_qk[0][p], R_qk[1][p]], writes=[Rbank[sbk[h]]])
                for h in range(2):
                    S.op("act", lambda e, h=h, ab=ab, sbk=sbk, c0=c0: e.activation(
                        out=scr[ab][:, h, c0:512], in_=banks[sbk[h]][:, c0:512], func=AF.Exp),
                        reads=[Rbank[sbk[h]]], writes=[R_scrh[ab][h]])
                    S.op("dve", lambda e, h=h, ab=ab, g=g, c0=c0: e.tensor_tensor(
                        PT[ab][:, h, c0:512], scr[ab][:, h, c0:512], Emk[:, g, h, c0:512], ALU.mult),
                        reads=[R_scrh[ab][h], R_E], writes=[R_PT[ab][h]])
                return (jt, ab, ubk, qs, kbs)

            def att_back(st, g=g):
                jt, ab, ubk, qs, kbs = st
                for hh in range(4):
                    p, h = hh // 2, hh % 2
                    for us in range(2):
                        for n_, (kb, jk) in enumerate(kbs):
                            lhs = Vt[:, jk, hh * 64:(hh + 1) * 64] if us == 0 else ones[:, 0:64]
                            S.op("pe", lambda e, lhs=lhs, kb=kb, ab=ab, hh=hh, p=p, h=h, us=us, n_=n_, ubk=ubk, nk=len(kbs): e.matmul(
                                banks[ubk][64 * h:64 * h + 64, (p * 2 + us) * 128:(p * 2 + us + 1) * 128], lhs,
                                PT[ab][:, h, (kb * 2 + p) * 128:(kb * 2 + p + 1) * 128], start=(n_ == 0), stop=(n_ == nk - 1)),
                                reads=[R_PT[ab][h], R_V[jk]], writes=[Rbank[ubk]])
                uv = banks[ubk][:, :].rearrange("p (a b) -> p a b", a=4)
                if g == 0:
                    S.op("act", lambda e, uv=uv, qs=qs: e.copy(out=UTST[:, :, qs], in_=uv),
                         reads=[Rbank[ubk]], writes=[R_UG[0][jt]] + R_xst)
                else:
                    S.op("dve", lambda e, uv=uv, qs=qs: e.tensor_tensor(UTST[:, :, qs], uv, UTST[:, :, qs], ALU.add),
                         reads=[Rbank[ubk]] + R_UG[g - 1], writes=[R_UG[g][jt]])

            prev_st = None
            for jt in range(NT if CUT > 0.7 else 2):
                st = att_front(jt)
                if prev_st is not None:
                    att_back(prev_st)
                prev_st = st
            att_back(prev_st)
        S.barrier()
        for p in range(2):
            S.op("act", lambda e, p=p: e.activation(out=UTST[:, 2 * p + 1, :], in_=UTST[:, 2 * p + 1, :], func=AF.Ln),
                 reads=R_UG[2], writes=[R_UTST])
            S.op("act", lambda e, p=p: e.activation(out=UTST[:, 2 * p + 1, :], in_=UTST[:, 2 * p + 1, :], func=AF.Exp, scale=-1.0),
                 reads=[R_UTST], writes=[R_UTST])
            S.op("dve", lambda e, p=p: e.tensor_tensor(AT[:, p, :], UTST[:, 2 * p, :], UTST[:, 2 * p + 1, :], ALU.mult),
                 reads=[R_UTST], writes=[R_AT])
        if dbg and b == 0:
            S.barrier()
            dma("sp", dbg_o["AT"].ap(), AT.rearrange("p a b -> p (a b)"), "dbg")
        if stage <= 1:
            break
        S.barrier()

        R_u = [Res("u0"), Res("u1")]
        R_s = [Res("s0"), Res("s1")]
        R_diff = [Res("d0"), Res("d1")]
        R_BT = Res("BT")
        sl = 0
        if stage >= 3:
            dma("pool", wg[1][:, :, 0, :], w_in_v[:, :, GA_OFF: GA_OFF + 256], "w1", writes=[Rw[1]])
            dma("pool", wg[1][:, :, 1, :], w_in_v[:, :, GB_OFF: GB_OFF + 256], "w1", writes=[Rw[1]])
        def p2_front(n_gi, gi):
            ub = n_gi % 2
            for tc in range(4):
                bk = next_ipbank()
                for c in range(8):
                    S.op("pe", lambda e, bk=bk, c=c, gi=gi, tc=tc, sl=sl: e.matmul(
                        banks[bk][:, :], wu[sl][:, c, gi * 128:(gi + 1) * 128], xT[:, c, tc * 512:(tc + 1) * 512],
                        start=(c == 0), stop=(c == 7)), reads=[Rw[sl]], writes=[Rbank[bk]])
                col = POOL_OFF // 128 + gi
                S.op("act", lambda e, bk=bk, ub=ub, tc=tc, col=col: e.activation(
                    out=ubuf[ub][:, tc * 512:(tc + 1) * 512], in_=banks[bk][:, :], func=AF.Identity,
                    bias=bcol[:, col:col + 1], scale=1.0), reads=[Rbank[bk]], writes=[R_u[ub]])
            w = 2 << gi
            cur, Rcur = ubuf[ub], R_u[ub]
            k = 0
            step = 1
            while step < w:
                nxt, Rn = sbuf2[k], R_s[k]
                S.op("dve", lambda e, nxt=nxt, cur=cur, step=step: e.tensor_tensor(
                    nxt[:, step:], cur[:, step:], cur[:, :T - step], ALU.add), reads=[Rcur], writes=[Rn])
                S.op("dve", lambda e, nxt=nxt, cur=cur, step=step: e.tensor_copy(nxt[:, :step], cur[:, :step]),
                     reads=[Rcur], writes=[Rn])
                cur, Rcur = nxt, Rn
                k ^= 1
                step *= 2
            S.op("dve", lambda e, cur=cur, ub=ub, w=w: e.scalar_tensor_tensor(
                diffT[ub][:, :], cur[:, :], 1.0 / w, ubuf[ub][:, :], ALU.mult, ALU.subtract),
                reads=[Rcur, R_u[ub]], writes=[R_diff[ub]])
            S.op("dve", lambda e, cur=cur, w=w: e.tensor_tensor(tmp16[:, :w - 1], cur[:, :w - 1], invc[:, :w - 1], ALU.mult),
                 reads=[Rcur], writes=[R_diff[ub]])
            S.op("dve", lambda e, ub=ub, w=w: e.tensor_tensor(diffT[ub][:, :w - 1], tmp16[:, :w - 1], ubuf[ub][:, :w - 1], ALU.subtract),
                 reads=[R_u[ub]], writes=[R_diff[ub]])

        def p2_back(n_gi, gi):
            ub = n_gi % 2
            for tc in range(4):
                bk = 3 + (tc % 2)
                S.op("pe", lambda e, bk=bk, gi=gi, ub=ub, tc=tc: e.matmul(
                    banks[bk][:, :], wpool[:, gi, :], diffT[ub][:, tc * 512:(tc + 1) * 512], start=True, stop=True),
                    reads=[R_diff[ub]], writes=[Rbank[bk]])
                S.op("act", lambda e, bk=bk, gi=gi, tc=tc: e.activation(
                    out=BT[:, gi, tc * 512:(tc + 1) * 512], in_=banks[bk][:, :], func=AF.Identity,
                    scale=pscale[:, gi:gi + 1]), reads=[Rbank[bk]], writes=[R_BT])

        p2_order = (3, 2, 1, 0)
        for n_gi, gi in enumerate(p2_order):
            p2_front(n_gi, gi)
            if n_gi > 0:
                p2_back(n_gi - 1, p2_order[n_gi - 1])
        p2_back(3, p2_order[3])
        if dbg and b == 0:
            S.barrier()
            dma("sp", dbg_o["BT"].ap(), BT.rearrange("p a b -> p (a b)"), "dbg")
        if stage <= 2:
            break
        S.barrier()

        R_G = [Res("G0"), Res("G1")]
        R_t12 = [Res("t0"), Res("t1")]
        R_mixed = [Res("mx%d" % d_) for d_ in range(8)]
        R_xtok = [Res("xt%d" % k_) for k_ in range(4)]
        R_r = [Res("r0"), Res("r1")]
        R_x1bf = [Res("xb0"), Res("xb1"), Res("xb2")]
        R_x1T = [Res("x1T%d" % i) for i in range(NT)]
        R_x1s = [Res("x1s%d" % i) for i in range(NT)]
        R_log = Res("logits")
        R_st = [Res("st0"), Res("st1")]
        R_st4 = [Res("st4_%d" % k_) for k_ in range(4)]

        def ln1_load(i):
            sl4 = i % 4
            dma("sp", xtok[sl4][:, :], x[b, i * 128:(i + 1) * 128, :], "xtok%d" % sl4, writes=[R_xtok[sl4]])

        def ln1_Y(i):
            sub = i % 4
            for half in range(2):
                for d_ in range(8):
                    S.op("pe", lambda e, half=half, d_=d_, sub=sub: e.matmul(
                        banks[4 + half][:, :], mixedT[:, d_, sub * 128:(sub + 1) * 128],
                        Wout[:, d_, half * 512:(half + 1) * 512], start=(d_ == 0), stop=(d_ == 7)),
                        reads=[R_mixed[d_]], writes=[Rbank[4 + half]])

        def ln1_A(i):
            sl4 = i % 4
            for half in range(2):
                hs = slice(half * 512, (half + 1) * 512)
                S.op("dve", lambda e, half=half, hs=hs, sl4=sl4: e.scalar_tensor_tensor(
                    xtok[sl4][:, hs], xtok[sl4][:, hs], ALPHA, banks[4 + half][:, :], ALU.mult, ALU.add),
                    reads=[Rbank[4 + half]], writes=[R_xtok[sl4]])
                S.op("dve", lambda e, half=half, hs=hs, sl4=sl4: e.bn_stats(stats[:, sl4, half, :], xtok[sl4][:, hs]),
                     reads=[R_xtok[sl4]], writes=[R_st4[sl4]])
            S.op("dve", lambda e, sl4=sl4: e.bn_aggr(mv[:, sl4, :], stats[:, sl4, :, :]), reads=[R_st4[sl4]], writes=[R_st4[sl4]])
            S.op("act", lambda e, sl4=sl4: e.activation(out=rstd[:, sl4, 0:1], in_=mv[:, sl4, 1:2], func=AF.Sqrt, bias=epsc[:, 0:1], scale=1.0),
                 reads=[R_st4[sl4]], writes=[R_st4[sl4]])

        def ln1_B(i):
            sl4 = i % 4
            S.op("dve", lambda e, sl4=sl4: e.reciprocal(rstd[:, sl4, 0:1], rstd[:, sl4, 0:1]),
                 reads=[R_st4[sl4]], writes=[R_st4[sl4]])
            S.op("dve", lambda e, sl4=sl4: e.scalar_tensor_tensor(
                rstd[:, sl4, 1:2], mv[:, sl4, 0:1], -1.0, rstd[:, sl4, 0:1], ALU.mult, ALU.mult),
                reads=[R_st4[sl4]], writes=[R_st4[sl4]])
            S.op("act", lambda e, sl4=sl4: e.activation(
                out=xtok[sl4][:, :], in_=xtok[sl4][:, :], func=AF.Identity, bias=rstd[:, sl4, 1:2], scale=rstd[:, sl4, 0:1]),
                reads=[R_st4[sl4]], writes=[R_xtok[sl4]])

        def ln1_C(i):
            sl4 = i % 4
            x3 = i % 3
            S.op("dve", lambda e, sl4=sl4: e.tensor_tensor(xtok[sl4][:, :], xtok[sl4][:, :], gam1[:, :], ALU.mult),
                 writes=[R_xtok[sl4]])
            S.op("dve", lambda e, sl4=sl4: e.tensor_tensor(xtok[sl4][:, :], xtok[sl4][:, :], bet1[:, :], ALU.add),
                 writes=[R_xtok[sl4]])
            S.op("act", lambda e, sl4=sl4, x3=x3: e.copy(out=x1bf[x3][:, :], in_=xtok[sl4][:, :]),
                 reads=[R_xtok[sl4]], writes=[R_x1bf[x3]])
            S.op("dve", lambda e, sl4=sl4: e.tensor_scalar(xtok[sl4][:, :], xtok[sl4][:, :], ALPHA, None, ALU.mult),
                 writes=[R_xtok[sl4]])
            dma("sp", x1s[i * 128:(i + 1) * 128, :], xtok[sl4][:, :], "x1st%d" % sl4, reads=[R_xtok[sl4]], writes=[R_x1s[i]])

        def ln1_TRp(i):
            x3 = i % 3
            tp6 = bank_bf(6)
            for c in range(8):
                S.op("pe", lambda e, c=c, x3=x3, tp6=tp6: e.transpose(tp6[:, c, :], x1bf[x3][:, c * 128:(c + 1) * 128], ident[:, :]),
                     reads=[R_x1bf[x3]], writes=[Rbank[6]])
            S.op("act", lambda e, i=i, tp6=tp6: e.copy(out=x1T[:, :, i * 128:(i + 1) * 128], in_=tp6),
                 reads=[Rbank[6]], writes=[R_x1T[i]])

        def ln1_RT(i):
            for c in range(8):
                S.op("pe", lambda e, c=c, i=i: e.matmul(
                    banks[7][:, 0:36], x1T[:, c, i * 128:(i + 1) * 128], Wr[:, c, :], start=(c == 0), stop=False),
                    reads=[R_x1T[i]], writes=[Rbank[7]])
            S.op("pe", lambda e: e.matmul(banks[7][:, 0:36], ones[0:1, :], brrow[0:1, :], start=False, stop=True),
                 writes=[Rbank[7]])

        def ln1_LG(i):
            S.op("act", lambda e, i=i: e.copy(out=logits[:, i, :], in_=banks[7][:, 0:36]),
                 reads=[Rbank[7]], writes=[R_log])

        def ln1_step(i):
            ok = lambda j: 0 <= j < NT
            if ok(i - 3):
                ln1_TRp(i - 3)
            if ok(i):
                ln1_Y(i)
            if ok(i - 3):
                ln1_RT(i - 3)
            if ok(i - 2):
                ln1_C(i - 2)
            if ok(i - 1):
                ln1_B(i - 1)
            if ok(i):
                ln1_A(i)
            if ok(i - 3):
                ln1_LG(i - 3)
            if ok(i + 1):
                ln1_load(i + 1)

        gctr = 0
        for tc in range(4):
            tsl = slice(tc * 512, (tc + 1) * 512)
            for dp in range(4):
                sl = (1 + 4 * tc + dp) % 2
                if not (tc == 0 and dp == 0):
                    dma("pool", wg[sl][:, :, 0, :], w_in_v[:, :, GA_OFF + 256 * dp: GA_OFF + 256 * (dp + 1)], "w%d" % sl, writes=[Rw[sl]])
                    dma("pool", wg[sl][:, :, 1, :], w_in_v[:, :, GB_OFF + 256 * dp: GB_OFF + 256 * (dp + 1)], "w%d" % sl, writes=[Rw[sl]])
                for dd in range(2):
                    d_ = 2 * dp + dd
                    gs = gctr % 2
                    gctr += 1
                    for gt in range(2):
                        for c in range(8):
                            S.op("pe", lambda e, gt=gt, c=c, dd=dd, sl=sl, tsl=tsl: e.matmul(
                                banks[gt][:, :], wg[sl][:, c, gt, dd * 128:(dd + 1) * 128], xT[:, c, tsl],
                                start=(c == 0), stop=(c == 7)), reads=[Rw[sl]], writes=[Rbank[gt]])
                        col = (GA_OFF if gt == 0 else GB_OFF) // 128 + d_
                        S.op("act", lambda e, gt=gt, gs=gs, col=col: e.activation(
                            out=G[gs][:, gt, :], in_=banks[gt][:, :], func=AF.Sigmoid, bias=bcol[:, col:col + 1], scale=1.0),
                            reads=[Rbank[gt]], writes=[R_G[gs]])
                    for p in range(2):
                        S.op("pe", lambda e, p=p, d_=d_, tsl=tsl: e.matmul(
                            banks[2][:, :], Pa[:, p, d_ * 128:(d_ + 1) * 128], AT[:, p, tsl], start=(p == 0), stop=(p == 1)),
                            reads=[R_AT], writes=[Rbank[2]])
                    for gi in range(4):
                        S.op("pe", lambda e, gi=gi, d_=d_, tsl=tsl: e.matmul(
                            banks[3][:, :], Pb[:, gi, d_ * 128:(d_ + 1) * 128], BT[:, gi, tsl], start=(gi == 0), stop=(gi == 3)),
                            reads=[R_BT], writes=[Rbank[3]])
                    for gt in range(2):
                        S.op("dve", lambda e, gt=gt, gs=gs: e.tensor_tensor(
                            t12[gs][:, gt, :], banks[2 + gt][:, :], G[gs][:, gt, :], ALU.mult),
                            reads=[Rbank[2 + gt], R_G[gs]], writes=[R_t12[gs]])
                    S.op("dve", lambda e, gs=gs, d_=d_: e.tensor_tensor(
                        mixedT[:, d_, :], t12[gs][:, 0, :], t12[gs][:, 1, :], ALU.add),
                        reads=[R_t12[gs]], writes=[R_mixed[d_]])
            if tc == 0:
                ln1_load(0)
            for sub in range(4):
                ln1_step(4 * tc + sub)
        for i_ in range(NT, NT + 3):
            ln1_step(i_)
        S.barrier()
        R_we = [Res("we0"), Res("we1")]
        R_sg = [Res("sg0"), Res("sg1")]
        R_h = [Res("h0"), Res("h1")]
        R_acc = [Res("acc%d" % i) for i in range(NT)]
        R_xtok2 = [Res("x20"), Res("x21")]
        ybanks = [(4, 5), (6, 7)]
        yctr = [0]

        R_wd = [Res("wd0"), Res("wd1")]

        def load_gu(e_):
            sl_ = (e_ + 1) % 2
            dma("pool", wexp_g[sl_], weg_d[e_].rearrange("(c p) f -> p c f", p=128), "we%d" % sl_, writes=[R_we[sl_]])
            dma("pool", wexp_u[sl_], weu_d[e_].rearrange("(c p) f -> p c f", p=128), "we%d" % sl_, writes=[R_we[sl_]])

        def load_d(e_):
            sl_ = (e_ + 1) % 2
            dma("pool", wexp_d[sl_], wed_d[e_].rearrange("(c p) n -> p c n", p=128), "wd%d" % sl_, writes=[R_wd[sl_]])

        load_gu(0)
        load_d(0)
        R_rt = Res("route")
        gl = logits[:, :, 0:4]
        el = logits[:, :, 4:36]
        gmax, gsum, gprob, m1, m2, e21, w1, w2, den = (rsm[:, k_, :] for k_ in range(9))
        gsh = rbig[:, 0, :, 0:4]
        gex = rbig[:, 0, :, 4:8]
        pen = rbig[:, 0, :, 8:12]
        elm = rbig[:, 1, :, :]
        elm2 = rbig[:, 2, :, :]
        eq = rbig[:, 0, :, :]

        def bc(a, n):
            return a.unsqueeze(2).to_broadcast([128, NT, n])

        def rop(eng, fn):
            S.op(eng, fn, reads=[R_log, R_rt], writes=[R_rt])

        rop("dve", lambda e: e.tensor_reduce(gmax, gl, AX.X, ALU.max))
        rop("dve", lambda e: e.tensor_tensor(gsh, gl, bc(gmax, 4), ALU.subtract))
        rop("act", lambda e: e.activation(out=gex, in_=gsh, func=AF.Exp))
        rop("dve", lambda e: e.tensor_reduce(gsum, gex, AX.X, ALU.add))
        rop("dve", lambda e: e.reciprocal(gprob, gsum))
        rop("dve", lambda e: e.tensor_scalar(pen, gsh, 0.0, -1e30, ALU.not_equal, ALU.mult))
        rop("dve", lambda e: e.tensor_tensor(
            elm.rearrange("p t (g k) -> p t g k", g=4), el.rearrange("p t (g k) -> p t g k", g=4),
            pen.unsqueeze(3).to_broadcast([128, NT, 4, 8]), ALU.add))
        rop("dve", lambda e: e.tensor_reduce(m1, elm, AX.X, ALU.max))
        rop("dve", lambda e: e.tensor_tensor(elm2, elm, bc(m1, 32), ALU.is_equal))
        rop("dve", lambda e: e.scalar_tensor_tensor(elm2, elm2, -1e30, elm, ALU.mult, ALU.add))
        rop("dve", lambda e: e.tensor_reduce(m2, elm2, AX.X, ALU.max))
        rop("dve", lambda e: e.tensor_tensor(e21, m2, m1, ALU.subtract))
        rop("act", lambda e: e.activation(out=e21, in_=e21, func=AF.Exp))
        rop("dve", lambda e: e.tensor_scalar(den, e21, 1.0, None, ALU.add))
        rop("dve", lambda e: e.reciprocal(den, den))
        rop("dve", lambda e: e.tensor_tensor(w1, den, gprob, ALU.mult))
        rop("dve", lambda e: e.tensor_tensor(w2, w1, e21, ALU.mult))
        rop("dve", lambda e: e.tensor_tensor(eq, elm, bc(m1, 32), ALU.is_equal))
        rop("dve", lambda e: e.tensor_tensor(call, eq, bc(w1, 32), ALU.mult))
        rop("dve", lambda e: e.tensor_tensor(eq, elm2, bc(m2, 32), ALU.is_equal))
        rop("dve", lambda e: e.tensor_tensor(eq, eq, bc(w2, 32), ALU.mult))
        rop("dve", lambda e: e.tensor_tensor(call, call, eq, ALU.add))
        if dbg and b == 0:
            S.barrier()
            dma("sp", dbg_o["call"].ap(), call.rearrange("p a b -> p (a b)"), "dbg")
        if stage <= 3:
            break
        S.barrier()

        def gate_up(e_, tc):
            sl_ = (e_ + 1) % 2
            tsl = slice(tc * 512, (tc + 1) * 512)
            for gu in range(2):
                wt = wexp_g[sl_] if gu == 0 else wexp_u[sl_]
                for fc in range(2):
                    bk = gu * 2 + fc
                    for c in range(8):
                        S.op("pe", lambda e, bk=bk, wt=wt, fc=fc, c=c, tsl=tsl: e.matmul(
                            banks[bk][:, :], wt[:, c, fc * 128:(fc + 1) * 128], x1T[:, c, tsl], start=(c == 0), stop=(c == 7)),
                            reads=[R_we[sl_]], writes=[Rbank[bk]])
            hb = (e_ * 4 + tc) % 2
            for fc in range(2):
                S.op("act", lambda e, fc=fc, hb=hb: e.activation(out=sg[hb][:, fc, :], in_=banks[fc][:, :], func=AF.Silu),
                     reads=[Rbank[fc]], writes=[R_sg[hb]])
                S.op("dve", lambda e, fc=fc, hb=hb: e.tensor_tensor(hT[hb][:, fc, :], banks[2 + fc][:, :], sg[hb][:, fc, :], ALU.mult),
                     reads=[Rbank[2 + fc], R_sg[hb]], writes=[R_h[hb]])

        def down(e_, tc):
            sl_ = (e_ + 1) % 2
            hb = (e_ * 4 + tc) % 2
            for sub in range(4):
                i = 4 * tc + sub
                yb = ybanks[yctr[0] % 2]
                yctr[0] += 1
                for half in range(2):
                    for fc in range(2):
                        S.op("pe", lambda e, yb=yb, half=half, fc=fc, hb=hb, sub=sub, sl_=sl_: e.matmul(
                            banks[yb[half]][:, :], hT[hb][:, fc, sub * 128:(sub + 1) * 128],
                            wexp_d[sl_][:, fc, half * 512:(half + 1) * 512], start=(fc == 0), stop=(fc == 1)),
                            reads=[R_h[hb], R_wd[sl_]], writes=[Rbank[yb[half]]])
                    hs = slice(half * 512, (half + 1) * 512)
                    if e_ == 0:
                        par = i % 2
                        if half == 0:
                            dma("sp", xtok2[par][:, :], x1s[i * 128:(i + 1) * 128, :], "xtok2%d" % par,
                                reads=[R_x1s[i]], writes=[R_xtok2[par]])
                        S.op("dve", lambda e, yb=yb, half=half, hs=hs, i=i, e_=e_, par=par: e.scalar_tensor_tensor(
                            acc[:, i, hs], banks[yb[half]][:, :], call[:, i, e_:e_ + 1], xtok2[par][:, hs], ALU.mult, ALU.add),
                            reads=[Rbank[yb[half]], R_xtok2[par]], writes=[R_acc[i]])
                    else:
                        S.op("dve", lambda e, yb=yb, half=half, hs=hs, i=i, e_=e_: e.scalar_tensor_tensor(
                            acc[:, i, hs], banks[yb[half]][:, :], call[:, i, e_:e_ + 1], acc[:, i, hs], ALU.mult, ALU.add),
                            reads=[Rbank[yb[half]], R_acc[i]], writes=[R_acc[i]])

        def ln2_A(i):
            par = i % 2
            q4 = i % 4
            for half in range(2):
                hs = slice(half * 512, (half + 1) * 512)
                S.op("dve", lambda e, hs=hs, q4=q4, i=i, half=half: e.bn_stats(stats[:, q4, half, :], acc[:, i, hs]),
                     reads=[R_acc[i]], writes=[R_st4[q4]])
            S.op("dve", lambda e, q4=q4: e.bn_aggr(mv[:, q4, :], stats[:, q4, :, :]), reads=[R_st4[q4]], writes=[R_st4[q4]])
            S.op("act", lambda e, q4=q4: e.activation(out=rstd[:, q4, 0:1], in_=mv[:, q4, 1:2], func=AF.Sqrt, bias=epsc[:, 0:1], scale=1.0),
                 reads=[R_st4[q4]], writes=[R_st4[q4]])

        def ln2_B(i):
            q4 = i % 4
            S.op("dve", lambda e, q4=q4: e.reciprocal(rstd[:, q4, 0:1], rstd[:, q4, 0:1]),
                 reads=[R_st4[q4]], writes=[R_st4[q4]])
            S.op("dve", lambda e, q4=q4: e.scalar_tensor_tensor(
                rstd[:, q4, 1:2], mv[:, q4, 0:1], -1.0, rstd[:, q4, 0:1], ALU.mult, ALU.mult),
                reads=[R_st4[q4]], writes=[R_st4[q4]])
            S.op("act", lambda e, q4=q4, i=i: e.activation(
                out=acc[:, i, :], in_=acc[:, i, :], func=AF.Identity, bias=rstd[:, q4, 1:2], scale=rstd[:, q4, 0:1]),
                reads=[R_st4[q4], R_acc[i]], writes=[R_acc[i]])

        def ln2_C(i):
            S.op("pool", lambda e, i=i: e.tensor_tensor(acc[:, i, :], acc[:, i, :], gam2[:, :], ALU.mult),
                 reads=[R_acc[i]], writes=[R_acc[i]])
            S.op("dve", lambda e, i=i: e.tensor_tensor(acc[:, i, :], acc[:, i, :], bet2[:, :], ALU.add),
                 reads=[R_acc[i]], writes=[R_acc[i]])
            dma("sp", out[b, i * 128:(i + 1) * 128, :], acc[:, i, :], "outst", reads=[R_acc[i]], writes=[R_acc[i]])

        def ln2_chunk(tc_):
            for k_ in range(6):
                if k_ < 4:
                    ln2_A(4 * tc_ + k_)
                if 0 <= k_ - 1 < 4:
                    ln2_B(4 * tc_ + k_ - 1)
                if 0 <= k_ - 2 < 4:
                    ln2_C(4 * tc_ + k_ - 2)

        seq = [(e_, tc) for e_ in range(NEXP) for tc in range(4)]
        for n_, (e_, tc) in enumerate(seq):
            if tc == 0 and e_ + 1 < NEXP:
                load_gu(e_ + 1)
            gate_up(e_, tc)
            if n_ > 0:
                down(*seq[n_ - 1])
                if seq[n_ - 1][0] == NEXP - 1 and not dbg:
                    ln2_chunk(seq[n_ - 1][1])
            if tc == 0 and e_ + 1 < NEXP:
                load_d(e_ + 1)
        down(*seq[-1])
        if not dbg:
            ln2_chunk(3)
        else:
            if b == 0:
                S.barrier()
                dma("sp", dbg_o["moe"].ap().rearrange("t p d -> p t d"), acc, "dbg")
                S.barrier()
            for tc_ in range(4):
                ln2_chunk(tc_)
        S.barrier()

    S.barrier()
    S.emit(nc)
    return nc


def _t5_bucket(dist):
    dist = np.asarray(dist, dtype=np.int32)
    max_exact = 16
    d = np.maximum(dist, 1).astype(np.float32)
    large = max_exact + (np.log(d / np.float32(max_exact)) / np.float32(math.log(2048 / max_exact))
                         * np.float32(32 - max_exact)).astype(np.int32)
    large = np.minimum(large, 31)
    return np.where(dist < max_exact, dist, large)


def _host_constants(rel_bias_table):
    k = np.arange(128)[:, None]
    q = np.arange(128)[None, :]
    biasg = np.zeros((128, 3, 2, 2, 2, 128), np.float32)
    maskg = np.zeros((128, 3, 2, 2, 2, 128), np.float32)
    for g in range(3):
        for kb in range(2):
            step = q + 128 - (k + 128 * kb)
            valid = (step >= 0) & (step <= 128)
            bucket = _t5_bucket(np.clip(step, 0, 128) * DIL[g])
            for p in range(2):
                for h in range(2):
                    biasg[:, g, h, kb, p, :] = rel_bias_table[bucket, g * 4 + 2 * p + h]
                    maskg[:, g, h, kb, p, :] = valid
    return biasg.reshape(128, -1), maskg.reshape(128, -1)


_PROG = {}


def _get_prog(key=(2, 99, False)):
    if key not in _PROG:
        _PROG[key] = build_program(*key)
    return _PROG[key]


def make_in_maps(inputs):
    f = lambda a: np.ascontiguousarray(np.asarray(a, dtype=np.float32))
    x = f(inputs["x"])
    b_in = f(inputs["b_in"])[0]
    biasg, maskg = _host_constants(f(inputs["rel_bias_table"]))
    common = {
        "w_in": f(inputs["w_in"])[0],
        "bcol": np.ascontiguousarray(b_in.reshape(38, 128).T),
        "bvrow": np.ascontiguousarray(b_in[V_OFF:POOL_OFF].reshape(1, 768)),
        "biasg": biasg,
        "maskg": maskg,
        "ident": np.eye(128, dtype=np.float32),
        "invc": np.ascontiguousarray(np.broadcast_to(1.0 / np.arange(1, 17, dtype=np.float32), (128, 16))),
        "w_pool": f(inputs["w_pool"])[0],
        "pscale": np.ascontiguousarray(f(inputs["pool_scale"])[0].reshape(4, 128).T),
        "w_proj_attn": f(inputs["w_proj_attn"])[0],
        "w_proj_pool": f(inputs["w_proj_pool"])[0],
        "w_out": f(inputs["w_out"])[0],
        "ln1_gamma": f(inputs["ln1_gamma"])[0],
        "ln1_beta": f(inputs["ln1_beta"])[0],
        "ln2_gamma": f(inputs["ln2_gamma"])[0],
        "ln2_beta": f(inputs["ln2_beta"])[0],
        "w_router": np.ascontiguousarray(np.concatenate(
            [f(inputs["w_router_group"])[0], f(inputs["w_router_expert"])[0]], axis=1)),
        "brrow": np.ascontiguousarray(np.concatenate(
            [f(inputs["b_router_group"])[0], f(inputs["b_router_expert"])[0]]).reshape(1, 36)),
        "w_expert_gate": f(inputs["w_expert_gate"])[0],
        "w_expert_up": f(inputs["w_expert_up"])[0],
        "w_expert_down": f(inputs["w_expert_down"])[0],
    }
    maps = []
    for c in range(N_CORES):
        m = dict(common)
        m["x"] = np.ascontiguousarray(x[2 * c:2 * c + 2])
        maps.append(m)
    return maps


def kernel(**inputs):
    nc = _get_prog()
    in_maps = make_in_maps(inputs)
    res = run_bass_kernel_spmd(nc, in_maps, core_ids=list(range(N_CORES)))
    return np.concatenate([np.asarray(r["out"]) for r in res.results], axis=0).astype(np.float32)
```

```python
import math
import numpy as np
import ml_dtypes
import concourse.bass as bass
import concourse.mybir as mybir
from concourse.bass_utils import run_bass_kernel_spmd

F32 = mybir.dt.float32
BF16 = mybir.dt.bfloat16
AF = mybir.ActivationFunctionType
ALU = mybir.AluOpType
AX = mybir.AxisListType

D = 1024
T = 2048
NT = 16
Q_OFF, K_OFF, V_OFF, POOL_OFF, GA_OFF, GB_OFF, IN_W = 0, 768, 1536, 2304, 2816, 3840, 4864
NEXP = 32
DEXP = 256
ALPHA = (2.0 * 1) ** 0.25
LN_EPS = 1e-5
DIL = (1, 4, 16)
N_CORES = 8
CUT = 99.0


class Res:
    __slots__ = ("name", "w", "rs")

    def __init__(self, name):
        self.name = name
        self.w = None
        self.rs = []


class Sched:
    ENG = ("pe", "act", "dve", "pool", "sp")

    def __init__(self):
        self.ops = {e: [] for e in self.ENG}
        self.seen = {e: {} for e in self.ENG}
        self.clock = {}
        self.chan = {}
        self.pending = {e: [] for e in self.ENG}

    def _need(self, eng, tok, waits):
        key, val = tok
        if key == eng and eng == "pe":
            return
        if self.seen[eng].get(key, -1) >= val:
            return
        waits.append(tok)
        if key in self.ENG:
            self.ops[key][val]["sig"] = True
        sn = self.seen[eng]
        for k, v in self.clock[tok].items():
            if sn.get(k, -1) < v:
                sn[k] = v

    def op(self, eng, fn, reads=(), writes=(), chan=None):
        waits = []
        toks = self.pending[eng]
        self.pending[eng] = []
        for r in reads:
            if r.w is not None:
                toks.append(r.w)
        for w in writes:
            if w.w is not None:
                toks.append(w.w)
            toks.extend(w.rs)
        for t in toks:
            self._need(eng, t, waits)
        idx = len(self.ops[eng])
        self.ops[eng].append(dict(fn=fn, waits=waits, sig=False, chan=chan))
        if chan is None:
            tok = (eng, idx)
        else:
            self.chan[chan] = self.chan.get(chan, 0) + 1
            tok = (chan, self.chan[chan])
        snap = dict(self.seen[eng])
        if snap.get(tok[0], -1) < tok[1]:
            snap[tok[0]] = tok[1]
        self.clock[tok] = snap
        for r in reads:
            r.rs.append(tok)
        for w in writes:
            w.w = tok
            w.rs = []
        return tok

    def barrier(self):
        toks = []
        for e in self.ENG:
            for i in range(len(self.ops[e]) - 1, -1, -1):
                if self.ops[e][i]["chan"] is None:
                    toks.append((e, i))
                    break
        for c, v in self.chan.items():
            toks.append((c, v))
        for e in self.ENG:
            self.pending[e].extend(toks)

    def emit(self, nc):
        trailing = {}
        for e in self.ENG:
            w = []
            for t in self.pending[e]:
                if t[0] == e:
                    continue
                self._need(e, t, w)
            self.pending[e] = []
            trailing[e] = w
        sems = {}
        for e in self.ENG:
            if e != "sp":
                sems[e] = nc.alloc_semaphore("sem_" + e)
        for c in self.chan:
            sems[c] = nc.alloc_semaphore("dma_" + c)
        sigcount = {}
        for e in self.ENG:
            cnt = 0
            m = []
            for rec in self.ops[e]:
                if rec["sig"]:
                    cnt += 1
                m.append(cnt)
            sigcount[e] = m
        self.sig_totals = {e: (sigcount[e][-1] if sigcount[e] else 0) for e in self.ENG}

        def run(name, eng):
            def wait(tok):
                key, val = tok
                if key in self.ENG:
                    eng.wait_ge(sems[key], sigcount[key][val])
                else:
                    eng.wait_ge(sems[key], 16 * val)

            for rec in self.ops[name]:
                for tok in rec["waits"]:
                    wait(tok)
                ins = rec["fn"](eng)
                if rec["chan"] is not None:
                    ins.then_inc(sems[rec["chan"]], 16)
                elif rec["sig"]:
                    ins.then_inc(sems[name], 1)
            for tok in trailing[name]:
                wait(tok)

        with nc.Block() as block:
            @block.sync
            def _(e):
                run("sp", e)

            @block.tensor
            def _(e):
                run("pe", e)

            @block.scalar
            def _(e):
                run("act", e)

            @block.vector
            def _(e):
                run("dve", e)

            @block.gpsimd
            def _(e):
                run("pool", e)


def tokset(g, j):
    if g == 0:
        return slice(128 * j, 128 * j + 128)
    if g == 1:
        r, n = j // 4, j % 4
        return slice(512 * n + r, 512 * (n + 1), 4)
    return slice(j, T, 16)


def prev_tile(g, j):
    if g == 0:
        return j - 1 if j > 0 else None
    if g == 1:
        return j - 1 if (j % 4) > 0 else None
    return None


def build_program(nb=2, stage=99, dbg=False):
    nc = bass.Bass("TRN2", target_bir_lowering=False)
    S = Sched()

    def din(name, shape, dt=F32):
        return nc.dram_tensor(name, list(shape), dt, kind="ExternalInput")

    x = din("x", [2, T, D])
    w_in = din("w_in", [D, IN_W])
    bcol_d = din("bcol", [128, 38])
    bvrow_d = din("bvrow", [1, 768])
    biasg_d = din("biasg", [128, 3 * 2 * 4 * 128])
    maskg_d = din("maskg", [128, 3 * 2 * 4 * 128])
    ident_d = din("ident", [128, 128])
    invc_d = din("invc", [128, 16])
    wpool_d = din("w_pool", [4, 128, 128])
    pscale_d = din("pscale", [128, 4])
    pa_d = din("w_proj_attn", [256, D])
    pb_d = din("w_proj_pool", [512, D])
    wout_d = din("w_out", [D, D])
    g1_d, b1_d = din("ln1_gamma", [D]), din("ln1_beta", [D])
    g2_d, b2_d = din("ln2_gamma", [D]), din("ln2_beta", [D])
    wr_d = din("w_router", [D, 36])
    brrow_d = din("brrow", [1, 36])
    nexp_decl = NEXP if stage >= 4 else 1
    weg_d = din("w_expert_gate", [nexp_decl, D, DEXP])
    weu_d = din("w_expert_up", [nexp_decl, D, DEXP])
    wed_d = din("w_expert_down", [nexp_decl, DEXP, D])
    out = nc.dram_tensor("out", [2, T, D], F32, kind="ExternalOutput")
    x1s = nc.dram_tensor("x1s", [T, D], F32, kind="ExternalOutput" if dbg else "Internal")
    dbg_o = {}
    if dbg:
        dbg_o["AT"] = nc.dram_tensor("dbg_AT", [128, 2 * T], BF16, kind="ExternalOutput")
        dbg_o["BT"] = nc.dram_tensor("dbg_BT", [128, 4 * T], BF16, kind="ExternalOutput")
        dbg_o["call"] = nc.dram_tensor("dbg_call", [128, NT * 32], F32, kind="ExternalOutput")
        dbg_o["moe"] = nc.dram_tensor("dbg_moe", [NT, 128, D], F32, kind="ExternalOutput")

    w_in_v = w_in.ap().rearrange("(c p) n -> p c n", p=128)

    def sb(name, shape, dt):
        return nc.alloc_sbuf_tensor("sb_" + name, list(shape), dt)

    ident = sb("ident", [128, 128], BF16)
    ones = sb("ones", [128, 128], BF16)
    bcol = sb("bcol", [128, 38], F32)
    bq8 = sb("bq8", [128, 6], F32)
    bvrow = sb("bvrow", [1, 768], BF16)
    pscale = sb("pscale", [128, 4], F32)
    invc = sb("invc", [128, 16], F32)
    gam1, bet1 = sb("gam1", [128, D], F32), sb("bet1", [128, D], F32)
    gam2, bet2 = sb("gam2", [128, D], F32), sb("bet2", [128, D], F32)
    Wout = sb("Wout", [128, 8, D], BF16)
    Pa = sb("Pa", [128, 2, D], BF16)
    Pb = sb("Pb", [128, 4, D], BF16)
    wpool = sb("wpool", [128, 4, 128], BF16)
    Wr = sb("Wr", [128, 8, 36], BF16)
    brrow = sb("brrow", [1, 36], BF16)
    stats = sb("stats", [128, 4, 2, 6], F32)
    mv = sb("mv", [128, 4, 2], F32)
    rstd = sb("rstd", [128, 4, 2], F32)
    tmp16 = sb("tmp16", [128, 16], F32)
    epsc = sb("epsc", [128, 1], F32)

    ARENA_KB = 156
    arena = nc.alloc_sbuf_tensor("arena", [128, ARENA_KB * 256], F32)

    def carve(off_kb, shape, dt):
        nbytes = int(np.prod(shape[1:])) * (4 if dt == F32 else 2)
        w0 = int(off_kb * 256)
        assert abs(w0 - off_kb * 256) < 1e-9 and nbytes % 4 == 0
        assert w0 * 4 + nbytes <= ARENA_KB * 1024, (off_kb, shape)
        ap = arena[:, w0:w0 + nbytes // 4]
        if dt != F32:
            ap = ap.bitcast(dt)
        if len(shape) == 3:
            ap = ap.rearrange("p (a b) -> p a b", a=shape[1])
        elif len(shape) == 4:
            ap = ap.rearrange("p (a b c) -> p a b c", a=shape[1], b=shape[2])
        elif len(shape) == 5:
            ap = ap.rearrange("p (a b c d) -> p a b c d", a=shape[1], b=shape[2], c=shape[3])
        return ap

    xT = carve(0, [128, 8, T], BF16)
    AT = carve(32, [128, 2, T], BF16)
    BT = carve(40, [128, 4, T], BF16)
    Emk = carve(40, [128, 3, 2, 512], F32)
    PT = [carve(52 + 2 * i, [128, 2, 512], BF16) for i in range(2)] + [carve(36, [128, 2, 512], BF16)]
    wq = [carve(56 + 12 * i, [128, 8, 768], BF16) for i in range(2)]
    wu = [carve(56 + 8 * i, [128, 8, 512], BF16) for i in range(2)]
    wg = [carve(56 + 8 * i, [128, 8, 2, 256], BF16) for i in range(2)]
    qk = [[carve(80 + 8 * j + 4 * p, [128, T], BF16) for p in range(2)] for j in range(2)]
    Vt = carve(96, [128, NT, 256], BF16)
    scr = [carve(104 + 4 * i, [128, 2, 512], F32) for i in range(2)] + [carve(32, [128, 2, 512], F32)]
    UTST = carve(112, [128, 4, T], F32)
    xbf = [carve(144 + 2 * i, [128, D], BF16) for i in range(4)]
    xst = [carve(128 + 4 * i, [128, D], F32) for i in range(4)]
    ubuf = [carve(72 + 8 * i, [128, T], F32) for i in range(2)]
    sbuf2 = [carve(88 + 8 * i, [128, T], F32) for i in range(2)]
    diffT = [carve(104 + 4 * i, [128, T], BF16) for i in range(2)]
    x1T = carve(72, [128, 8, T], BF16)
    G = [carve(104 + 4 * i, [128, 2, 512], F32) for i in range(2)]
    mixedT = carve(112, [128, 8, 512], BF16)
    t12 = [carve(120 + 4 * i, [128, 2, 512], F32) for i in range(2)]
    xtok = [carve(128 + 4 * i, [128, D], F32) for i in range(4)]
    x1bf = [carve(144 + 2 * i, [128, D], BF16) for i in range(2)] + [carve(153, [128, D], BF16)]
    logits = carve(148, [128, NT, 36], F32)
    rbig = carve(104, [128, 3, NT, 32], F32)
    rsm = carve(110, [128, 12, NT], F32)
    call = carve(150.5, [128, NT, 32], F32)
    acc = carve(0, [128, NT, D], F32)
    xtok2 = [carve(64 + 4 * i, [128, D], F32) for i in range(2)]
    wexp_g = [carve(104 + 12 * i, [128, 8, 256], BF16) for i in range(2)]
    wexp_u = [carve(108 + 12 * i, [128, 8, 256], BF16) for i in range(2)]
    wexp_d = [carve(112 + 12 * i, [128, 2, D], BF16) for i in range(2)]
    hT = [carve(128 + 2 * i, [128, 2, 512], BF16) for i in range(2)]
    sg = [carve(132 + 4 * i, [128, 2, 512], F32) for i in range(2)]

    banks = [nc.alloc_psum_tensor("bank%d" % i, [128, 512], F32) for i in range(8)]
    Rbank = [Res("bank%d" % i) for i in range(8)]

    def bank_bf(i):
        return banks[i][:, :].bitcast(BF16).rearrange("p (a b) -> p a b", a=8)

    def dma(eng, out_ap, in_ap, chan, reads=(), writes=()):
        return S.op(eng, lambda e: e.dma_start(out=out_ap, in_=in_ap), reads=reads, writes=writes, chan=chan + "_" + eng)

    dma("sp", bcol[:, :], bcol_d.ap(), "const")
    dma("sp", pscale[:, :], pscale_d.ap(), "const")
    dma("sp", invc[:, :], invc_d.ap(), "const")
    for t, d_ in ((gam1, g1_d), (bet1, b1_d), (gam2, g2_d), (bet2, b2_d)):
        dma("sp", t[:, :], d_.ap().partition_broadcast(128), "const")
    dma("pool", ident[:, :], ident_d.ap(), "const")
    dma("pool", bvrow[:, :], bvrow_d.ap(), "const")
    dma("pool", brrow[:, :], brrow_d.ap(), "const")
    dma("pool", Wout[:, :, :], wout_d.ap().rearrange("(c p) n -> p c n", p=128), "const")
    dma("pool", Pa[:, :, :], pa_d.ap().rearrange("(c p) n -> p c n", p=128), "const")
    dma("pool", Pb[:, :, :], pb_d.ap().rearrange("(c p) n -> p c n", p=128), "const")
    dma("pool", wpool[:, :, :], wpool_d.ap().rearrange("g c d -> c g d"), "const")
    dma("pool", Wr[:, :, :], wr_d.ap().rearrange("(c p) n -> p c n", p=128), "const")
    S.op("dve", lambda e: e.memset(ones[:, :], 1.0))
    S.op("dve", lambda e: e.memset(epsc[:, :], LN_EPS))
    S.barrier()
    S.op("dve", lambda e: e.tensor_scalar(bq8[:, :], bcol[:, 0:6], 0.125, None, ALU.mult))
    S.barrier()

    wslot = [0]
    Rw = [Res("w0"), Res("w1")]
    ipb = [0]

    def next_ipbank():
        ipb[0] ^= 1
        return 1 + ipb[0]

    for b in range(nb):
        if CUT <= 0.1:
            break
        R_xbf = [Res("xbf%d" % i) for i in range(4)]
        R_xT = [Res("xT%d" % i) for i in range(NT)]
        R_E = Res("E")
        R_scr = [Res("scr0"), Res("scr1")]
        R_scrh = [[Res("scrh%d%d" % (i, h)) for h in range(2)] for i in range(3)]
        R_PT = [[Res("PT%d%d" % (i, h)) for h in range(2)] for i in range(3)]
        R_UG = [[Res("UG%d_%d" % (g_, i)) for i in range(NT)] for g_ in range(3)]
        R_qk = [[Res("qk%d%d" % (j, p)) for p in range(2)] for j in range(2)]
        R_V = [Res("V%d" % i) for i in range(NT)]
        R_UTST = Res("UTST")
        R_AT = Res("AT")
        tpv = bank_bf(0)
        R_xst = [Res("xst%d" % i) for i in range(4)]
        for i in range(NT):
            sl = i % 4
            dma("sp", xst[sl][:, :], x[b, i * 128:(i + 1) * 128, :], "xst%d" % sl, writes=[R_xst[sl]])
            S.op("dve", lambda e, sl=sl: e.tensor_copy(xbf[sl][:, :], xst[sl][:, :]), reads=[R_xst[sl]], writes=[R_xbf[sl]])
            for c in range(8):
                S.op("pe", lambda e, c=c, sl=sl: e.transpose(tpv[:, c, :], xbf[sl][:, c * 128:(c + 1) * 128], ident[:, :]),
                     reads=[R_xbf[sl]], writes=[Rbank[0]])
            S.op("act", lambda e, i=i: e.copy(out=xT[:, :, i * 128:(i + 1) * 128], in_=tpv),
                 reads=[Rbank[0]], writes=[R_xT[i]])

        if CUT <= 0.2:
            break
        for g in range(3):
            for kb in range(2):
                o = (g * 2 + kb) * 512
                sc = scr[kb][:, 0, :]
                dma("sp", sc, biasg_d[:, o:o + 512], "escr%d" % kb, writes=[R_scrh[kb][0]])
                dma("sp", Emk[:, g, kb, :], maskg_d[:, o:o + 512], "emask", writes=[R_E])
                S.op("act", lambda e, sc=sc: e.activation(out=sc, in_=sc, func=AF.Exp),
                     reads=[R_scrh[kb][0]], writes=[R_scrh[kb][0]])
                S.op("dve", lambda e, sc=sc, g=g, kb=kb: e.tensor_tensor(Emk[:, g, kb, :], Emk[:, g, kb, :], sc, ALU.mult),
                     reads=[R_scrh[kb][0], R_E], writes=[R_E])

        att_ctr = [0]
        wslot[0] = 0
        if CUT <= 0.3:
            break
        for g in range(3 if CUT > 0.9 else 1):
            sl = wslot[0] % 2
            wslot[0] += 1
            for j, off in enumerate((Q_OFF, K_OFF, V_OFF)):
                dma("pool", wq[sl][:, :, j * 256:(j + 1) * 256],
                    w_in_v[:, :, off + g * 256: off + (g + 1) * 256], "w%d" % sl, writes=[Rw[sl]])
            for j in range(2):
                for p in range(2):
                    for tc in range(4):
                        bk = next_ipbank()
                        for c in range(8):
                            S.op("pe", lambda e, bk=bk, c=c, j=j, p=p, tc=tc, sl=sl: e.matmul(
                                banks[bk][:, :], wq[sl][:, c, j * 256 + p * 128: j * 256 + (p + 1) * 128],
                                xT[:, c, tc * 512:(tc + 1) * 512], start=(c == 0), stop=(c == 7)),
                                reads=[Rw[sl]] + R_xT[4 * tc:4 * tc + 4], writes=[Rbank[bk]])
                        col = (Q_OFF if j == 0 else K_OFF) // 128 + g * 2 + p
                        if j == 0:
                            S.op("act", lambda e, bk=bk, p=p, tc=tc, col=col: e.activation(
                                out=qk[0][p][:, tc * 512:(tc + 1) * 512], in_=banks[bk][:, :], func=AF.Identity,
                                bias=bq8[:, col:col + 1], scale=0.125), reads=[Rbank[bk]], writes=[R_qk[0][p]])
                        else:
                            S.op("act", lambda e, bk=bk, p=p, tc=tc, col=col: e.activation(
                                out=qk[1][p][:, tc * 512:(tc + 1) * 512], in_=banks[bk][:, :], func=AF.Identity,
                                bias=bcol[:, col:col + 1], scale=1.0), reads=[Rbank[bk]], writes=[R_qk[1][p]])
            for jt in range(NT):
                bk = next_ipbank()
                ts = tokset(g, jt)
                for c in range(8):
                    S.op("pe", lambda e, bk=bk, c=c, ts=ts, sl=sl: e.matmul(
                        banks[bk][:, 0:256], xT[:, c, ts], wq[sl][:, c, 512:768], start=(c == 0), stop=False),
                        reads=[Rw[sl]] + R_xT, writes=[Rbank[bk]])
                S.op("pe", lambda e, bk=bk, g=g: e.matmul(
                    banks[bk][:, 0:256], ones[0:1, :], bvrow[0:1, g * 256:(g + 1) * 256], start=False, stop=True),
                    writes=[Rbank[bk]])
                S.op("dve", lambda e, bk=bk, jt=jt: e.tensor_copy(Vt[:, jt, :], banks[bk][:, 0:256]),
                     reads=[Rbank[bk]], writes=[R_V[jt]])

            if g == 2:
                dma("pool", wu[0][:, :, :], w_in_v[:, :, POOL_OFF:POOL_OFF + 512], "w0", writes=[Rw[0]])
            if CUT <= 0.5:
                break

            def att_front(jt, g=g):
                ab = att_ctr[0] % 3
                sbk = (3, 4) if att_ctr[0] % 2 == 0 else (5, 6)
                ubk = (7, 0, 1)[ab]
                att_ctr[0] += 1
                qs = tokset(g, jt)
                kbs = []
                pj = prev_tile(g, jt)
                if pj is not None:
                    kbs.append((0, pj))
                kbs.append((1, jt))
                c0 = 0 if len(kbs) == 2 else 256
                for kb, jk in kbs:
                    ks = tokset(g, jk)
                    for p in range(2):
                        for h in range(2):
                            cb = (kb * 2 + p) * 128
                            S.op("pe", lambda e, cb=cb, ks=ks, qs=qs, p=p, h=h, sbk=sbk: e.matmul(
                                banks[sbk[h]][:, cb:cb + 128], qk[1][p][64 * h:64 * h + 64, ks],
                                qk[0][p][64 * h:64 * h + 64, qs], start=True, stop=True),
                                reads=[R_qk[0][p], R_qk[1][p]], writes=[Rbank[sbk[h]]])
                for h in range(2):
                    S.op("act", lambda e, h=h, ab=ab, sbk=sbk, c0=c0: e.activation(
                        out=scr[ab][:, h, c0:512], in_=banks[sbk[h]][:, c0:512], func=AF.Exp),
                        reads=[Rbank[sbk[h]]], writes=[R_scrh[ab][h]])
                    S.op("dve", lambda e, h=h, ab=ab, g=g, c0=c0: e.tensor_tensor(
                        PT[ab][:, h, c0:512], scr[ab][:, h, c0:512], Emk[:, g, h, c0:512], ALU.mult),
                        reads=[R_scrh[ab][h], R_E], writes=[R_PT[ab][h]])
                return (jt, ab, ubk, qs, kbs)

            def att_back(st, g=g):
                jt, ab, ubk, qs, kbs = st
                for hh in range(4):
                    p, h = hh // 2, hh % 2
                    for us in range(2):
                        for n_, (kb, jk) in enumerate(kbs):
                            lhs = Vt[:, jk, hh * 64:(hh + 1) * 64] if us == 0 else ones[:, 0:64]
                            S.op("pe", lambda e, lhs=lhs, kb=kb, ab=ab, hh=hh, p=p, h=h, us=us, n_=n_, ubk=ubk, nk=len(kbs): e.matmul(
                                banks[ubk][64 * h:64 * h + 64, (p * 2 + us) * 128:(p * 2 + us + 1) * 128], lhs,
                                PT[ab][:, h, (kb * 2 + p) * 128:(kb * 2 + p + 1) * 128], start=(n_ == 0), stop=(n_ == nk - 1)),
                                reads=[R_PT[ab][h], R_V[jk]], writes=[Rbank[ubk]])
                uv = banks[ubk][:, :].rearrange("p (a b) -> p a b", a=4)
                if g == 0:
                    S.op("act", lambda e, uv=uv, qs=qs: e.copy(out=UTST[:, :, qs], in_=uv),
                         reads=[Rbank[ubk]], writes=[R_UG[0][jt]] + R_xst)
                else:
                    S.op("dve", lambda e, uv=uv, qs=qs: e.tensor_tensor(UTST[:, :, qs], uv, UTST[:, :, qs], ALU.add),
                         reads=[Rbank[ubk]] + R_UG[g - 1], writes=[R_UG[g][jt]])

            prev_st = None
            for jt in range(NT if CUT > 0.7 else 2):
                st = att_front(jt)
                if prev_st is not None:
                    att_back(prev_st)
                prev_st = st
            att_back(prev_st)
        S.barrier()
        for p in range(2):
            S.op("act", lambda e, p=p: e.activation(out=UTST[:, 2 * p + 1, :], in_=UTST[:, 2 * p + 1, :], func=AF.Ln),
                 reads=R_UG[2], writes=[R_UTST])
            S.op("act", lambda e, p=p: e.activation(out=UTST[:, 2 * p + 1, :], in_=UTST[:, 2 * p + 1, :], func=AF.Exp, scale=-1.0),
                 reads=[R_UTST], writes=[R_UTST])
            S.op("dve", lambda e, p=p: e.tensor_tensor(AT[:, p, :], UTST[:, 2 * p, :], UTST[:, 2 * p + 1, :], ALU.mult),
                 reads=[R_UTST], writes=[R_AT])
        if dbg and b == 0:
            S.barrier()
            dma("sp", dbg_o["AT"].ap(), AT.rearrange("p a b -> p (a b)"), "dbg")
        if stage <= 1:
            break
        S.barrier()

        R_u = [Res("u0"), Res("u1")]
        R_s = [Res("s0"), Res("s1")]
        R_diff = [Res("d0"), Res("d1")]
        R_BT = Res("BT")
        sl = 0
        if stage >= 3:
            dma("pool", wg[1][:, :, 0, :], w_in_v[:, :, GA_OFF: GA_OFF + 256], "w1", writes=[Rw[1]])
            dma("pool", wg[1][:, :, 1, :], w_in_v[:, :, GB_OFF: GB_OFF + 256], "w1", writes=[Rw[1]])
        def p2_front(n_gi, gi):
            ub = n_gi % 2
            for tc in range(4):
                bk = next_ipbank()
                for c in range(8):
                    S.op("pe", lambda e, bk=bk, c=c, gi=gi, tc=tc, sl=sl: e.matmul(
                        banks[bk][:, :], wu[sl][:, c, gi * 128:(gi + 1) * 128], xT[:, c, tc * 512:(tc + 1) * 512],
                        start=(c == 0), stop=(c == 7)), reads=[Rw[sl]], writes=[Rbank[bk]])
                col = POOL_OFF // 128 + gi
                S.op("act", lambda e, bk=bk, ub=ub, tc=tc, col=col: e.activation(
                    out=ubuf[ub][:, tc * 512:(tc + 1) * 512], in_=banks[bk][:, :], func=AF.Identity,
                    bias=bcol[:, col:col + 1], scale=1.0), reads=[Rbank[bk]], writes=[R_u[ub]])
            w = 2 << gi
            cur, Rcur = ubuf[ub], R_u[ub]
            k = 0
            step = 1
            while step < w:
                nxt, Rn = sbuf2[k], R_s[k]
                S.op("dve", lambda e, nxt=nxt, cur=cur, step=step: e.tensor_tensor(
                    nxt[:, step:], cur[:, step:], cur[:, :T - step], ALU.add), reads=[Rcur], writes=[Rn])
                S.op("dve", lambda e, nxt=nxt, cur=cur, step=step: e.tensor_copy(nxt[:, :step], cur[:, :step]),
                     reads=[Rcur], writes=[Rn])
                cur, Rcur = nxt, Rn
                k ^= 1
                step *= 2
            S.op("dve", lambda e, cur=cur, ub=ub, w=w: e.scalar_tensor_tensor(
                diffT[ub][:, :], cur[:, :], 1.0 / w, ubuf[ub][:, :], ALU.mult, ALU.subtract),
                reads=[Rcur, R_u[ub]], writes=[R_diff[ub]])
            S.op("dve", lambda e, cur=cur, w=w: e.tensor_tensor(tmp16[:, :w - 1], cur[:, :w - 1], invc[:, :w - 1], ALU.mult),
                 reads=[Rcur], writes=[R_diff[ub]])
            S.op("dve", lambda e, ub=ub, w=w: e.tensor_tensor(diffT[ub][:, :w - 1], tmp16[:, :w - 1], ubuf[ub][:, :w - 1], ALU.subtract),
                 reads=[R_u[ub]], writes=[R_diff[ub]])

        def p2_back(n_gi, gi):
            ub = n_gi % 2
            for tc in range(4):
                bk = 3 + (tc % 2)
                S.op("pe", lambda e, bk=bk, gi=gi, ub=ub, tc=tc: e.matmul(
                    banks[bk][:, :], wpool[:, gi, :], diffT[ub][:, tc * 512:(tc + 1) * 512], start=True, stop=True),
                    reads=[R_diff[ub]], writes=[Rbank[bk]])
                S.op("act", lambda e, bk=bk, gi=gi, tc=tc: e.activation(
                    out=BT[:, gi, tc * 512:(tc + 1) * 512], in_=banks[bk][:, :], func=AF.Identity,
                    scale=pscale[:, gi:gi + 1]), reads=[Rbank[bk]], writes=[R_BT])

        p2_order = (3, 2, 1, 0)
        for n_gi, gi in enumerate(p2_order):
            p2_front(n_gi, gi)
            if n_gi > 0:
                p2_back(n_gi - 1, p2_order[n_gi - 1])
        p2_back(3, p2_order[3])
        if dbg and b == 0:
            S.barrier()
            dma("sp", dbg_o["BT"].ap(), BT.rearrange("p a b -> p (a b)"), "dbg")
        if stage <= 2:
            break
        S.barrier()

        R_G = [Res("G0"), Res("G1")]
        R_t12 = [Res("t0"), Res("t1")]
        R_mixed = [Res("mx%d" % d_) for d_ in range(8)]
        R_xtok = [Res("xt%d" % k_) for k_ in range(4)]
        R_r = [Res("r0"), Res("r1")]
        R_x1bf = [Res("xb0"), Res("xb1"), Res("xb2")]
        R_x1T = [Res("x1T%d" % i) for i in range(NT)]
        R_x1s = [Res("x1s%d" % i) for i in range(NT)]
        R_log = Res("logits")
        R_st = [Res("st0"), Res("st1")]
        R_st4 = [Res("st4_%d" % k_) for k_ in range(4)]

        def ln1_load(i):
            sl4 = i % 4
            dma("sp", xtok[sl4][:, :], x[b, i * 128:(i + 1) * 128, :], "xtok%d" % sl4, writes=[R_xtok[sl4]])

        def ln1_Y(i):
            sub = i % 4
            for half in range(2):
                for d_ in range(8):
                    S.op("pe", lambda e, half=half, d_=d_, sub=sub: e.matmul(
                        banks[4 + half][:, :], mixedT[:, d_, sub * 128:(sub + 1) * 128],
                        Wout[:, d_, half * 512:(half + 1) * 512], start=(d_ == 0), stop=(d_ == 7)),
                        reads=[R_mixed[d_]], writes=[Rbank[4 + half]])

        def ln1_A(i):
            sl4 = i % 4
            for half in range(2):
                hs = slice(half * 512, (half + 1) * 512)
                S.op("dve", lambda e, half=half, hs=hs, sl4=sl4: e.scalar_tensor_tensor(
                    xtok[sl4][:, hs], xtok[sl4][:, hs], ALPHA, banks[4 + half][:, :], ALU.mult, ALU.add),
                    reads=[Rbank[4 + half]], writes=[R_xtok[sl4]])
                S.op("dve", lambda e, half=half, hs=hs, sl4=sl4: e.bn_stats(stats[:, sl4, half, :], xtok[sl4][:, hs]),
                     reads=[R_xtok[sl4]], writes=[R_st4[sl4]])
            S.op("dve", lambda e, sl4=sl4: e.bn_aggr(mv[:, sl4, :], stats[:, sl4, :, :]), reads=[R_st4[sl4]], writes=[R_st4[sl4]])
            S.op("act", lambda e, sl4=sl4: e.activation(out=rstd[:, sl4, 0:1], in_=mv[:, sl4, 1:2], func=AF.Sqrt, bias=epsc[:, 0:1], scale=1.0),
                 reads=[R_st4[sl4]], writes=[R_st4[sl4]])

        def ln1_B(i):
            sl4 = i % 4
            S.op("dve", lambda e, sl4=sl4: e.reciprocal(rstd[:, sl4, 0:1], rstd[:, sl4, 0:1]),
                 reads=[R_st4[sl4]], writes=[R_st4[sl4]])
            S.op("dve", lambda e, sl4=sl4: e.scalar_tensor_tensor(
                rstd[:, sl4, 1:2], mv[:, sl4, 0:1], -1.0, rstd[:, sl4, 0:1], ALU.mult, ALU.mult),
                reads=[R_st4[sl4]], writes=[R_st4[sl4]])
            S.op("act", lambda e, sl4=sl4: e.activation(
                out=xtok[sl4][:, :], in_=xtok[sl4][:, :], func=AF.Identity, bias=rstd[:, sl4, 1:2], scale=rstd[:, sl4, 0:1]),
                reads=[R_st4[sl4]], writes=[R_xtok[sl4]])

        def ln1_C(i):
            sl4 = i % 4
            x3 = i % 3
            S.op("dve", lambda e, sl4=sl4: e.tensor_tensor(xtok[sl4][:, :], xtok[sl4][:, :], gam1[:, :], ALU.mult),
                 writes=[R_xtok[sl4]])
            S.op("dve", lambda e, sl4=sl4: e.tensor_tensor(xtok[sl4][:, :], xtok[sl4][:, :], bet1[:, :], ALU.add),
                 writes=[R_xtok[sl4]])
            S.op("act", lambda e, sl4=sl4, x3=x3: e.copy(out=x1bf[x3][:, :], in_=xtok[sl4][:, :]),
                 reads=[R_xtok[sl4]], writes=[R_x1bf[x3]])
            S.op("dve", lambda e, sl4=sl4: e.tensor_scalar(xtok[sl4][:, :], xtok[sl4][:, :], ALPHA, None, ALU.mult),
                 writes=[R_xtok[sl4]])
            dma("sp", x1s[i * 128:(i + 1) * 128, :], xtok[sl4][:, :], "x1st%d" % sl4, reads=[R_xtok[sl4]], writes=[R_x1s[i]])

        def ln1_TRp(i):
            x3 = i % 3
            tp6 = bank_bf(6)
            for c in range(8):
                S.op("pe", lambda e, c=c, x3=x3, tp6=tp6: e.transpose(tp6[:, c, :], x1bf[x3][:, c * 128:(c + 1) * 128], ident[:, :]),
                     reads=[R_x1bf[x3]], writes=[Rbank[6]])
            S.op("act", lambda e, i=i, tp6=tp6: e.copy(out=x1T[:, :, i * 128:(i + 1) * 128], in_=tp6),
                 reads=[Rbank[6]], writes=[R_x1T[i]])

        def ln1_RT(i):
            for c in range(8):
                S.op("pe", lambda e, c=c, i=i: e.matmul(
                    banks[7][:, 0:36], x1T[:, c, i * 128:(i + 1) * 128], Wr[:, c, :], start=(c == 0), stop=False),
                    reads=[R_x1T[i]], writes=[Rbank[7]])
            S.op("pe", lambda e: e.matmul(banks[7][:, 0:36], ones[0:1, :], brrow[0:1, :], start=False, stop=True),
                 writes=[Rbank[7]])

        def ln1_LG(i):
            S.op("act", lambda e, i=i: e.copy(out=logits[:, i, :], in_=banks[7][:, 0:36]),
                 reads=[Rbank[7]], writes=[R_log])

        def ln1_step(i):
            ok = lambda j: 0 <= j < NT
            if ok(i - 3):
                ln1_TRp(i - 3)
            if ok(i):
                ln1_Y(i)
            if ok(i - 3):
                ln1_RT(i - 3)
            if ok(i - 2):
                ln1_C(i - 2)
            if ok(i - 1):
                ln1_B(i - 1)
            if ok(i):
                ln1_A(i)
            if ok(i - 3):
                ln1_LG(i - 3)
            if ok(i + 1):
                ln1_load(i + 1)

        gctr = 0
        for tc in range(4):
            tsl = slice(tc * 512, (tc + 1) * 512)
            for dp in range(4):
                sl = (1 + 4 * tc + dp) % 2
                if not (tc == 0 and dp == 0):
                    dma("pool", wg[sl][:, :, 0, :], w_in_v[:, :, GA_OFF + 256 * dp: GA_OFF + 256 * (dp + 1)], "w%d" % sl, writes=[Rw[sl]])
                    dma("pool", wg[sl][:, :, 1, :], w_in_v[:, :, GB_OFF + 256 * dp: GB_OFF + 256 * (dp + 1)], "w%d" % sl, writes=[Rw[sl]])
                for dd in range(2):
                    d_ = 2 * dp + dd
                    gs = gctr % 2
                    gctr += 1
                    for gt in range(2):
                        for c in range(8):
                            S.op("pe", lambda e, gt=gt, c=c, dd=dd, sl=sl, tsl=tsl: e.matmul(
                                banks[gt][:, :], wg[sl][:, c, gt, dd * 128:(dd + 1) * 128], xT[:, c, tsl],
                                start=(c == 0), stop=(c == 7)), reads=[Rw[sl]], writes=[Rbank[gt]])
                        col = (GA_OFF if gt == 0 else GB_OFF) // 128 + d_
                        S.op("act", lambda e, gt=gt, gs=gs, col=col: e.activation(
                            out=G[gs][:, gt, :], in_=banks[gt][:, :], func=AF.Sigmoid, bias=bcol[:, col:col + 1], scale=1.0),
                            reads=[Rbank[gt]], writes=[R_G[gs]])
                    for p in range(2):
                        S.op("pe", lambda e, p=p, d_=d_, tsl=tsl: e.matmul(
                            banks[2][:, :], Pa[:, p, d_ * 128:(d_ + 1) * 128], AT[:, p, tsl], start=(p == 0), stop=(p == 1)),
                            reads=[R_AT], writes=[Rbank[2]])
                    for gi in range(4):
                        S.op("pe", lambda e, gi=gi, d_=d_, tsl=tsl: e.matmul(
                            banks[3][:, :], Pb[:, gi, d_ * 128:(d_ + 1) * 128], BT[:, gi, tsl], start=(gi == 0), stop=(gi == 3)),
                            reads=[R_BT], writes=[Rbank[3]])
                    for gt in range(2):
                        S.op("dve", lambda e, gt=gt, gs=gs: e.tensor_tensor(
                            t12[gs][:, gt, :], banks[2 + gt][:, :], G[gs][:, gt, :], ALU.mult),
                            reads=[Rbank[2 + gt], R_G[gs]], writes=[R_t12[gs]])
                    S.op("dve", lambda e, gs=gs, d_=d_: e.tensor_tensor(
                        mixedT[:, d_, :], t12[gs][:, 0, :], t12[gs][:, 1, :], ALU.add),
                        reads=[R_t12[gs]], writes=[R_mixed[d_]])
            if tc == 0:
                ln1_load(0)
            for sub in range(4):
                ln1_step(4 * tc + sub)
        for i_ in range(NT, NT + 3):
            ln1_step(i_)
        S.barrier()
        R_we = [Res("we0"), Res("we1")]
        R_sg = [Res("sg0"), Res("sg1")]
        R_h = [Res("h0"), Res("h1")]
        R_acc = [Res("acc%d" % i) for i in range(NT)]
        R_xtok2 = [Res("x20"), Res("x21")]
        ybanks = [(4, 5), (6, 7)]
        yctr = [0]

        R_wd = [Res("wd0"), Res("wd1")]

        def load_gu(e_):
            sl_ = (e_ + 1) % 2
            dma("pool", wexp_g[sl_], weg_d[e_].rearrange("(c p) f -> p c f", p=128), "we%d" % sl_, writes=[R_we[sl_]])
            dma("pool", wexp_u[sl_], weu_d[e_].rearrange("(c p) f -> p c f", p=128), "we%d" % sl_, writes=[R_we[sl_]])

        def load_d(e_):
            sl_ = (e_ + 1) % 2
            dma("pool", wexp_d[sl_], wed_d[e_].rearrange("(c p) n -> p c n", p=128), "wd%d" % sl_, writes=[R_wd[sl_]])

        load_gu(0)
        load_d(0)
        R_rt = Res("route")
        gl = logits[:, :, 0:4]
        el = logits[:, :, 4:36]
        gmax, gsum, gprob, m1, m2, e21, w1, w2, den = (rsm[:, k_, :] for k_ in range(9))
        gsh = rbig[:, 0, :, 0:4]
        gex = rbig[:, 0, :, 4:8]
        pen = rbig[:, 0, :, 8:12]
        elm = rbig[:, 1, :, :]
        elm2 = rbig[:, 2, :, :]
        eq = rbig[:, 0, :, :]

        def bc(a, n):
            return a.unsqueeze(2).to_broadcast([128, NT, n])

        def rop(eng, fn):
            S.op(eng, fn, reads=[R_log, R_rt], writes=[R_rt])

        rop("dve", lambda e: e.tensor_reduce(gmax, gl, AX.X, ALU.max))
        rop("dve", lambda e: e.tensor_tensor(gsh, gl, bc(gmax, 4), ALU.subtract))
        rop("act", lambda e: e.activation(out=gex, in_=gsh, func=AF.Exp))
        rop("dve", lambda e: e.tensor_reduce(gsum, gex, AX.X, ALU.add))
        rop("dve", lambda e: e.reciprocal(gprob, gsum))
        rop("dve", lambda e: e.tensor_scalar(pen, gsh, 0.0, -1e30, ALU.not_equal, ALU.mult))
        rop("dve", lambda e: e.tensor_tensor(
            elm.rearrange("p t (g k) -> p t g k", g=4), el.rearrange("p t (g k) -> p t g k", g=4),
            pen.unsqueeze(3).to_broadcast([128, NT, 4, 8]), ALU.add))
        rop("dve", lambda e: e.tensor_reduce(m1, elm, AX.X, ALU.max))
        rop("dve", lambda e: e.tensor_tensor(elm2, elm, bc(m1, 32), ALU.is_equal))
        rop("dve", lambda e: e.scalar_tensor_tensor(elm2, elm2, -1e30, elm, ALU.mult, ALU.add))
        rop("dve", lambda e: e.tensor_reduce(m2, elm2, AX.X, ALU.max))
        rop("dve", lambda e: e.tensor_tensor(e21, m2, m1, ALU.subtract))
        rop("act", lambda e: e.activation(out=e21, in_=e21, func=AF.Exp))
        rop("dve", lambda e: e.tensor_scalar(den, e21, 1.0, None, ALU.add))
        rop("dve", lambda e: e.reciprocal(den, den))
        rop("dve", lambda e: e.tensor_tensor(w1, den, gprob, ALU.mult))
        rop("dve", lambda e: e.tensor_tensor(w2, w1, e21, ALU.mult))
        rop("dve", lambda e: e.tensor_tensor(eq, elm, bc(m1, 32), ALU.is_equal))
        rop("dve", lambda e: e.tensor_tensor(call, eq, bc(w1, 32), ALU.mult))
        rop("dve", lambda e: e.tensor_tensor(eq, elm2, bc(m2, 32), ALU.is_equal))
        rop("dve", lambda e: e.tensor_tensor(eq, eq, bc(w2, 32), ALU.mult))
        rop("dve", lambda e: e.tensor_tensor(call, call, eq, ALU.add))
        if dbg and b == 0:
            S.barrier()
            dma("sp", dbg_o["call"].ap(), call.rearrange("p a b -> p (a b)"), "dbg")
        if stage <= 3:
            break
        S.barrier()

        def gate_up(e_, tc, part):
            sl_ = (e_ + 1) % 2
            tsl = slice(tc * 512, (tc + 1) * 512)
            hb = (e_ * 4 + tc) % 2
            gu = part
            wt = wexp_g[sl_] if gu == 0 else wexp_u[sl_]
            for fc in range(2):
                bk = gu * 2 + fc
                for c in range(8):
                    S.op("pe", lambda e, bk=bk, wt=wt, fc=fc, c=c, tsl=tsl: e.matmul(
                        banks[bk][:, :], wt[:, c, fc * 128:(fc + 1) * 128], x1T[:, c, tsl], start=(c == 0), stop=(c == 7)),
                        reads=[R_we[sl_]], writes=[Rbank[bk]])
            for fc in range(2):
                if part == 0:
                    S.op("act", lambda e, fc=fc, hb=hb: e.activation(out=sg[hb][:, fc, :], in_=banks[fc][:, :], func=AF.Silu),
                         reads=[Rbank[fc]], writes=[R_sg[hb]])
                else:
                    S.op("dve", lambda e, fc=fc, hb=hb: e.tensor_tensor(hT[hb][:, fc, :], banks[2 + fc][:, :], sg[hb][:, fc, :], ALU.mult),
                         reads=[Rbank[2 + fc], R_sg[hb]], writes=[R_h[hb]])

        def down(e_, tc, part):
            sl_ = (e_ + 1) % 2
            hb = (e_ * 4 + tc) % 2
            for sub in range(2 * part, 2 * part + 2):
                i = 4 * tc + sub
                yb = ybanks[yctr[0] % 2]
                yctr[0] += 1
                for half in range(2):
                    for fc in range(2):
                        S.op("pe", lambda e, yb=yb, half=half, fc=fc, hb=hb, sub=sub, sl_=sl_: e.matmul(
                            banks[yb[half]][:, :], hT[hb][:, fc, sub * 128:(sub + 1) * 128],
                            wexp_d[sl_][:, fc, half * 512:(half + 1) * 512], start=(fc == 0), stop=(fc == 1)),
                            reads=[R_h[hb], R_wd[sl_]], writes=[Rbank[yb[half]]])
                    hs = slice(half * 512, (half + 1) * 512)
                    if e_ == 0:
                        par = i % 2
                        if half == 0:
                            dma("sp", xtok2[par][:, :], x1s[i * 128:(i + 1) * 128, :], "xtok2%d" % par,
                                reads=[R_x1s[i]], writes=[R_xtok2[par]])
                        S.op("dve", lambda e, yb=yb, half=half, hs=hs, i=i, e_=e_, par=par: e.scalar_tensor_tensor(
                            acc[:, i, hs], banks[yb[half]][:, :], call[:, i, e_:e_ + 1], xtok2[par][:, hs], ALU.mult, ALU.add),
                            reads=[Rbank[yb[half]], R_xtok2[par]], writes=[R_acc[i]])
                    else:
                        S.op("dve", lambda e, yb=yb, half=half, hs=hs, i=i, e_=e_: e.scalar_tensor_tensor(
                            acc[:, i, hs], banks[yb[half]][:, :], call[:, i, e_:e_ + 1], acc[:, i, hs], ALU.mult, ALU.add),
                            reads=[Rbank[yb[half]], R_acc[i]], writes=[R_acc[i]])

        seq = [(e_, tc) for e_ in range(NEXP) for tc in range(4)]
        for n_, (e_, tc) in enumerate(seq):
            if tc == 0 and e_ + 1 < NEXP:
                load_gu(e_ + 1)
            gate_up(e_, tc, 0)
            if n_ > 0:
                down(*seq[n_ - 1], 0)
            gate_up(e_, tc, 1)
            if n_ > 0:
                down(*seq[n_ - 1], 1)
            if tc == 0 and e_ + 1 < NEXP:
                load_d(e_ + 1)
        down(*seq[-1], 0)
        down(*seq[-1], 1)

        if dbg and b == 0:
            S.barrier()
            dma("sp", dbg_o["moe"].ap().rearrange("t p d -> p t d"), acc, "dbg")
            S.barrier()
        def ln2_A(i):
            par = i % 2
            q4 = i % 4
            for half in range(2):
                hs = slice(half * 512, (half + 1) * 512)
                S.op("dve", lambda e, hs=hs, q4=q4, i=i, half=half: e.bn_stats(stats[:, q4, half, :], acc[:, i, hs]),
                     reads=[R_acc[i]], writes=[R_st4[q4]])
            S.op("dve", lambda e, q4=q4: e.bn_aggr(mv[:, q4, :], stats[:, q4, :, :]), reads=[R_st4[q4]], writes=[R_st4[q4]])
            S.op("act", lambda e, q4=q4: e.activation(out=rstd[:, q4, 0:1], in_=mv[:, q4, 1:2], func=AF.Sqrt, bias=epsc[:, 0:1], scale=1.0),
                 reads=[R_st4[q4]], writes=[R_st4[q4]])

        def ln2_B(i):
            q4 = i % 4
            S.op("dve", lambda e, q4=q4: e.reciprocal(rstd[:, q4, 0:1], rstd[:, q4, 0:1]),
                 reads=[R_st4[q4]], writes=[R_st4[q4]])
            S.op("dve", lambda e, q4=q4: e.scalar_tensor_tensor(
                rstd[:, q4, 1:2], mv[:, q4, 0:1], -1.0, rstd[:, q4, 0:1], ALU.mult, ALU.mult),
                reads=[R_st4[q4]], writes=[R_st4[q4]])
            S.op("act", lambda e, q4=q4, i=i: e.activation(
                out=acc[:, i, :], in_=acc[:, i, :], func=AF.Identity, bias=rstd[:, q4, 1:2], scale=rstd[:, q4, 0:1]),
                reads=[R_st4[q4], R_acc[i]], writes=[R_acc[i]])

        def ln2_C(i):
            S.op("pool", lambda e, i=i: e.tensor_tensor(acc[:, i, :], acc[:, i, :], gam2[:, :], ALU.mult),
                 reads=[R_acc[i]], writes=[R_acc[i]])
            S.op("dve", lambda e, i=i: e.tensor_tensor(acc[:, i, :], acc[:, i, :], bet2[:, :], ALU.add),
                 reads=[R_acc[i]], writes=[R_acc[i]])
            dma("sp", out[b, i * 128:(i + 1) * 128, :], acc[:, i, :], "outst", reads=[R_acc[i]], writes=[R_acc[i]])

        for i in range(NT + 2):
            if i < NT:
                ln2_A(i)
            if 0 <= i - 1 < NT:
                ln2_B(i - 1)
            if 0 <= i - 2 < NT:
                ln2_C(i - 2)
        S.barrier()

    S.barrier()
    S.emit(nc)
    return nc


def _t5_bucket(dist):
    dist = np.asarray(dist, dtype=np.int32)
    max_exact = 16
    d = np.maximum(dist, 1).astype(np.float32)
    large = max_exact + (np.log(d / np.float32(max_exact)) / np.float32(math.log(2048 / max_exact))
                         * np.float32(32 - max_exact)).astype(np.int32)
    large = np.minimum(large, 31)
    return np.where(dist < max_exact, dist, large)


def _host_constants(rel_bias_table):
    k = np.arange(128)[:, None]
    q = np.arange(128)[None, :]
    biasg = np.zeros((128, 3, 2, 2, 2, 128), np.float32)
    maskg = np.zeros((128, 3, 2, 2, 2, 128), np.float32)
    for g in range(3):
        for kb in range(2):
            step = q + 128 - (k + 128 * kb)
            valid = (step >= 0) & (step <= 128)
            bucket = _t5_bucket(np.clip(step, 0, 128) * DIL[g])
            for p in range(2):
                for h in range(2):
                    biasg[:, g, h, kb, p, :] = rel_bias_table[bucket, g * 4 + 2 * p + h]
                    maskg[:, g, h, kb, p, :] = valid
    return biasg.reshape(128, -1), maskg.reshape(128, -1)


_PROG = {}


def _get_prog(key=(2, 99, False)):
    if key not in _PROG:
        _PROG[key] = build_program(*key)
    return _PROG[key]


def make_in_maps(inputs):
    f = lambda a: np.ascontiguousarray(np.asarray(a, dtype=np.float32))
    x = f(inputs["x"])
    b_in = f(inputs["b_in"])[0]
    biasg, maskg = _host_constants(f(inputs["rel_bias_table"]))
    common = {
        "w_in": f(inputs["w_in"])[0],
        "bcol": np.ascontiguousarray(b_in.reshape(38, 128).T),
        "bvrow": np.ascontiguousarray(b_in[V_OFF:POOL_OFF].reshape(1, 768)),
        "biasg": biasg,
        "maskg": maskg,
        "ident": np.eye(128, dtype=np.float32),
        "invc": np.ascontiguousarray(np.broadcast_to(1.0 / np.arange(1, 17, dtype=np.float32), (128, 16))),
        "w_pool": f(inputs["w_pool"])[0],
        "pscale": np.ascontiguousarray(f(inputs["pool_scale"])[0].reshape(4, 128).T),
        "w_proj_attn": f(inputs["w_proj_attn"])[0],
        "w_proj_pool": f(inputs["w_proj_pool"])[0],
        "w_out": f(inputs["w_out"])[0],
        "ln1_gamma": f(inputs["ln1_gamma"])[0],
        "ln1_beta": f(inputs["ln1_beta"])[0],
        "ln2_gamma": f(inputs["ln2_gamma"])[0],
        "ln2_beta": f(inputs["ln2_beta"])[0],
        "w_router": np.ascontiguousarray(np.concatenate(
            [f(inputs["w_router_group"])[0], f(inputs["w_router_expert"])[0]], axis=1)),
        "brrow": np.ascontiguousarray(np.concatenate(
            [f(inputs["b_router_group"])[0], f(inputs["b_router_expert"])[0]]).reshape(1, 36)),
        "w_expert_gate": f(inputs["w_expert_gate"])[0],
        "w_expert_up": f(inputs["w_expert_up"])[0],
        "w_expert_down": f(inputs["w_expert_down"])[0],
    }
    maps = []
    for c in range(N_CORES):
        m = dict(common)
        m["x"] = np.ascontiguousarray(x[2 * c:2 * c + 2])
        maps.append(m)
    return maps


def kernel(**inputs):
    nc = _get_prog()
    in_maps = make_in_maps(inputs)
    res = run_bass_kernel_spmd(nc, in_maps, core_ids=list(range(N_CORES)))
    return np.concatenate([np.asarray(r["out"]) for r in res.results], axis=0).astype(np.float32)
```
